# Optimizing a Trainium2 kernel written in Bass

```python
import jax
import jax.numpy as jnp
from jax import lax
import numpy as np

D_MODEL = 1024
BATCH = 4
SEQ = 8192
DEPTH = 1

GLA_HEADS = 4
GLA_DK = 128
GLA_DV = 128
GLA_RANK = 16
GLA_TAU = 16.0
GLA_CHUNK = 16
DIL_PATTERNS = ((128, 1), (512, 4), (2048, 16))
DIL_GROUPS = 3
DIL_HEADS = 4
DIL_DH = 128
DIL_BLOCK = 128
N_GROUPS = 4
EXPERTS_PER_GROUP = 8
N_EXPERTS = N_GROUPS * EXPERTS_PER_GROUP
TOP_K = 2
D_EXPERT = 512
MOE_BLOCK = 128
EPS = 1e-6

GLA_QK_W = GLA_HEADS * GLA_DK
GLA_V_W = GLA_HEADS * GLA_DV
DIL_W = DIL_HEADS * DIL_DH
SPLIT_SIZES = (GLA_QK_W, GLA_QK_W, GLA_V_W, GLA_V_W, GLA_RANK) + (DIL_W,) * (3 * DIL_GROUPS)
IN_COLS = sum(SPLIT_SIZES)
SPLIT_POINTS = tuple(int(v) for v in np.cumsum(SPLIT_SIZES)[:-1])
ALIBI_SLOPES = tuple(2.0 ** (-8.0 * (i + 1) / (DIL_GROUPS * DIL_HEADS)) for i in range(DIL_GROUPS * DIL_HEADS))

kernel_name = 'hybrid_gla_dilated_attn_hier_moe'


def rms_norm(x, g):
    xf = x.astype(jnp.float32)
    y = xf * lax.rsqrt(jnp.mean(xf * xf, axis=-1, keepdims=True) + EPS)
    return (y * g.astype(jnp.float32)).astype(x.dtype)


def gla_chunked(q, k, v, log_a):
    B, S, H, DK = q.shape
    DV = v.shape[-1]
    C = GLA_CHUNK
    N = S // C
    def chunks(t):
        return t.astype(jnp.float32).reshape(B, N, C, H, t.shape[-1]).transpose(1, 0, 3, 2, 4)
    qc = chunks(q) * (DK ** -0.5)
    kc = chunks(k)
    vc = chunks(v)
    b = jnp.cumsum(chunks(log_a), axis=3)
    b_last = b[:, :, :, -1:, :]
    q_in = qc * jnp.exp(b)
    k_in = kc * jnp.exp(-b)
    k_end = kc * jnp.exp(b_last - b)
    causal = jnp.tril(jnp.ones((C, C), dtype=bool))
    att = jnp.where(causal, jnp.einsum('nbhik,nbhjk->nbhij', q_in, k_in), 0.0)
    o_intra = jnp.einsum('nbhij,nbhjv->nbhiv', att, vc)

    def step(state, xs):
        q_n, k_n, v_n, dec_n = xs
        o_n = jnp.einsum('bhik,bhkv->bhiv', q_n, state)
        state = dec_n[:, :, 0, :, None] * state + jnp.einsum('bhjk,bhjv->bhkv', k_n, v_n)
        return state, o_n

    state0 = jnp.zeros((B, H, DK, DV), jnp.float32)
    _, o_inter = lax.scan(step, state0, (q_in, k_end, vc, jnp.exp(b_last)))
    o = o_intra + o_inter
    return o.transpose(1, 0, 3, 2, 4).reshape(B, S, H, DV)


def dilated_group(q, k, v, slopes, window, dilation):
    B, S, H, E = q.shape
    L = S // dilation
    Q = DIL_BLOCK
    span = window // dilation
    nb = -(-L // Q)
    Lp = nb * Q

    def to_sub(t):
        t = jnp.swapaxes(t.reshape(B, L, dilation, H, E), 1, 2)
        return jnp.pad(t, ((0, 0), (0, 0), (0, Lp - L), (0, 0), (0, 0)))

    def kv_blocks(t):
        t = jnp.pad(to_sub(t), ((0, 0), (0, 0), (Q, 0), (0, 0), (0, 0))).reshape(B, dilation, nb + 1, Q, H, E)
        return jnp.concatenate([t[:, :, :-1], t[:, :, 1:]], axis=3)

    def from_sub(t):
        t = t.reshape((B, dilation, Lp) + t.shape[4:])[:, :, :L]
        t = jnp.swapaxes(t, 1, 2)
        return t.reshape((B, S) + t.shape[3:])

    qb = to_sub(q).reshape(B, dilation, nb, Q, H, E)
    kb = kv_blocks(k)
    vb = kv_blocks(v)
    s = jnp.einsum('bdnqhe,bdnkhe->bdnhqk', qb, kb, preferred_element_type=jnp.float32) * (E ** -0.5)
    dist = jnp.arange(Q)[:, None] + Q - jnp.arange(2 * Q)[None, :]
    key_pos = jnp.arange(nb)[:, None, None] * Q + jnp.arange(2 * Q)[None, None, :] - Q
    valid = (dist >= 0) & (dist <= span) & (key_pos >= 0)
    bias = -slopes[:, None, None] * (dilation * dist).astype(jnp.float32)
    s = jnp.where(valid[:, None], s + bias, -jnp.inf)
    m = jnp.max(s, axis=-1)
    p = jnp.exp(s - m[..., None])
    den = jnp.sum(p, axis=-1)
    den_t = jnp.swapaxes(den, -1, -2)
    o = jnp.einsum('bdnhqk,bdnkhe->bdnqhe', p, vb.astype(jnp.float32)) / den_t[..., None]
    return from_sub(o), from_sub(jnp.swapaxes(m, -1, -2)), from_sub(den_t)


def hier_moe(h, w_rg, b_rg, w_re, b_re, w_gate, w_up, w_down):
    B, S, D = h.shape
    T = B * S
    hf = h.reshape(T, D)
    g_logit = (hf @ w_rg).astype(jnp.float32) + b_rg
    g_prob = jax.nn.softmax(g_logit, axis=-1)
    g_sel = jnp.argmax(g_logit, axis=-1)
    g_p = jnp.take_along_axis(g_prob, g_sel[:, None], axis=-1)[:, 0]
    e_logit = ((hf @ w_re).astype(jnp.float32) + b_re).reshape(T, N_GROUPS, EXPERTS_PER_GROUP)
    e_logit = jnp.take_along_axis(e_logit, g_sel[:, None, None], axis=1)[:, 0]
    top_v, top_i = lax.top_k(e_logit, TOP_K)
    gate = jax.nn.softmax(top_v, axis=-1) * g_p[:, None]
    eid = g_sel[:, None] * EXPERTS_PER_GROUP + top_i

    A = T * TOP_K
    flat_e = eid.reshape(A).astype(jnp.int32)
    flat_w = gate.reshape(A)
    flat_t = jnp.repeat(jnp.arange(T, dtype=jnp.int32), TOP_K)
    order = jnp.argsort(flat_e)
    se, st, sw = flat_e[order], flat_t[order], flat_w[order]
    counts = jnp.bincount(flat_e, length=N_EXPERTS)
    starts = jnp.cumsum(counts) - counts
    pcounts = (counts + MOE_BLOCK - 1) // MOE_BLOCK * MOE_BLOCK
    pends = jnp.cumsum(pcounts)
    pstarts = pends - pcounts
    dest = pstarts[se] + jnp.arange(A, dtype=jnp.int32) - starts[se]
    P = A + N_EXPERTS * MOE_BLOCK
    NB = P // MOE_BLOCK
    row_t = jnp.full((P,), T, jnp.int32).at[dest].set(st)
    row_w = jnp.zeros((P,), jnp.float32).at[dest].set(sw)
    blk_e = jnp.minimum(jnp.searchsorted(pends, jnp.arange(NB, dtype=jnp.int32) * MOE_BLOCK, side='right'), N_EXPERTS - 1)
    h_pad = jnp.concatenate([hf, jnp.zeros((1, D), hf.dtype)], axis=0)
    xin = h_pad[row_t].reshape(NB, MOE_BLOCK, D)

    def expert_block(args):
        xb, e = args
        return (jax.nn.silu(xb @ w_gate[e]) * (xb @ w_up[e])) @ w_down[e]

    yb = lax.map(expert_block, (xin, blk_e)).reshape(P, D)
    out = jnp.zeros((T + 1, D), jnp.float32).at[row_t].add(yb.astype(jnp.float32) * row_w[:, None])[:T]
    return out.reshape(B, S, D).astype(h.dtype)


def setup_inputs(seed: int = 0) -> dict:
    key = jax.random.key(seed)
    ks = jax.random.split(key, 22)
    def nrm(k, shape, scale):
        return scale * jax.random.normal(k, shape, jnp.float32)
    L = DEPTH
    return {
        'x': nrm(ks[0], (BATCH, SEQ, D_MODEL), 1.0),
        'norm1_g': 1.0 + nrm(ks[1], (L, D_MODEL), 0.02),
        'w_in': nrm(ks[2], (L, D_MODEL, IN_COLS), D_MODEL ** -0.5),
        'w_gla_a2': nrm(ks[3], (L, GLA_RANK, GLA_QK_W), GLA_RANK ** -0.5),
        'b_gla_a': nrm(ks[4], (L, GLA_QK_W), 0.1),
        'gla_out_norm_g': 1.0 + nrm(ks[5], (L, GLA_DV), 0.02),
        'dil_q_norm_g': 1.0 + nrm(ks[6], (L, DIL_GROUPS, DIL_DH), 0.02),
        'dil_k_norm_g': 1.0 + nrm(ks[7], (L, DIL_GROUPS, DIL_DH), 0.02),
        'w_proj_gla': nrm(ks[8], (L, GLA_V_W, D_MODEL), GLA_V_W ** -0.5),
        'w_proj_attn': nrm(ks[9], (L, DIL_W, D_MODEL), DIL_W ** -0.5),
        'w_branch_gate': nrm(ks[10], (L, D_MODEL, 2 * D_MODEL), D_MODEL ** -0.5),
        'b_branch_gate': nrm(ks[11], (L, 2 * D_MODEL), 0.01),
        'w_out': nrm(ks[12], (L, D_MODEL, D_MODEL), D_MODEL ** -0.5),
        'norm2_g': 1.0 + nrm(ks[13], (L, D_MODEL), 0.02),
        'w_router_group': nrm(ks[14], (L, D_MODEL, N_GROUPS), D_MODEL ** -0.5),
        'b_router_group': nrm(ks[15], (L, N_GROUPS), 0.01),
        'w_router_expert': nrm(ks[16], (L, D_MODEL, N_EXPERTS), D_MODEL ** -0.5),
        'b_router_expert': nrm(ks[17], (L, N_EXPERTS), 0.01),
        'w_gate': nrm(ks[18], (L, N_EXPERTS, D_MODEL, D_EXPERT), D_MODEL ** -0.5),
        'w_up': nrm(ks[19], (L, N_EXPERTS, D_MODEL, D_EXPERT), D_MODEL ** -0.5),
        'w_down': nrm(ks[20], (L, N_EXPERTS, D_EXPERT, D_MODEL), D_EXPERT ** -0.5),
    }


def reference(x, norm1_g, w_in, w_gla_a2, b_gla_a, gla_out_norm_g, dil_q_norm_g, dil_k_norm_g,
              w_proj_gla, w_proj_attn, w_branch_gate, b_branch_gate, w_out, norm2_g,
              w_router_group, b_router_group, w_router_expert, b_router_expert, w_gate, w_up, w_down):
    B, S, D = x.shape
    slopes = jnp.asarray(ALIBI_SLOPES, jnp.float32).reshape(DIL_GROUPS, DIL_HEADS)
    def heads(t, e):
        return t.reshape(B, S, -1, e)
    for l in range(DEPTH):
        h = rms_norm(x, norm1_g[l])
        parts = jnp.split(h @ w_in[l], list(SPLIT_POINTS), axis=-1)
        gq, gk, gv, gr, ga_lr = parts[:5]
        log_a = jax.nn.log_sigmoid((ga_lr @ w_gla_a2[l] + b_gla_a[l]).astype(jnp.float32)) / GLA_TAU
        o_gla = gla_chunked(heads(gq, GLA_DK), heads(gk, GLA_DK), heads(gv, GLA_DV), heads(log_a, GLA_DK))
        o_gla = rms_norm(o_gla, gla_out_norm_g[l]).reshape(B, S, GLA_V_W) * jax.nn.silu(gr.astype(jnp.float32))
        y_gla = o_gla.astype(x.dtype) @ w_proj_gla[l]
        outs, maxes, dens = [], [], []
        for gi, (window, dilation) in enumerate(DIL_PATTERNS):
            q, k, v = parts[5 + 3 * gi: 8 + 3 * gi]
            q = rms_norm(heads(q, DIL_DH), dil_q_norm_g[l, gi])
            k = rms_norm(heads(k, DIL_DH), dil_k_norm_g[l, gi])
            o_g, m_g, d_g = dilated_group(q, k, heads(v, DIL_DH), slopes[gi], window, dilation)
            outs.append(o_g)
            maxes.append(m_g)
            dens.append(d_g)
        m_all = jnp.stack(maxes)
        wts = jnp.stack(dens) * jnp.exp(m_all - jnp.max(m_all, axis=0))
        o_att = jnp.sum(wts[..., None] * jnp.stack(outs), axis=0) / jnp.sum(wts, axis=0)[..., None]
        y_att = o_att.reshape(B, S, DIL_W).astype(x.dtype) @ w_proj_attn[l]
        g_gla, g_att = jnp.split(jax.nn.sigmoid(h @ w_branch_gate[l] + b_branch_gate[l]), 2, axis=-1)
        x = x + ((g_gla * y_gla + g_att * y_att) @ w_out[l]).astype(x.dtype)
        x = x + hier_moe(rms_norm(x, norm2_g[l]), w_router_group[l], b_router_group[l], w_router_expert[l],
                         b_router_expert[l], w_gate[l], w_up[l], w_down[l])
    return x
```

```python
import numpy as np
import concourse.bass as bass
import concourse.mybir as mybir
from concourse.bass_utils import run_bass_kernel_spmd
from contextlib import ExitStack

F32 = mybir.dt.float32; BF16 = mybir.dt.bfloat16; I32 = mybir.dt.int32
AF = mybir.ActivationFunctionType; ALU = mybir.AluOpType; AX = mybir.AxisListType

D = 1024; NTOK = 4096; MGS = 2048; IN_COLS = 6672; EPS = 1e-6
NEXP = 32; DEXP = 512; NBLK = 96; PROWS = NBLK * 128
SLOPES = [2.0 ** (-8.0 * (i + 1) / 12) for i in range(12)]
DILS = (1, 4, 16)


def ss(s, n, st):
    return slice(s, s + (n - 1) * st + 1, st)


class T:
    __slots__ = ("lw", "rd")

    def __init__(self):
        self.lw = {}; self.rd = {}


class Buf:
    def __init__(self, h):
        self.h = h; self.t = T()

    def __getitem__(self, k):
        return self.h[k]


class Eng:
    def __init__(self, fw, name, eng):
        self.name = name; self.eng = eng
        self.sem = fw.nc.alloc_semaphore("s_" + name); self.cnt = 0
        self.seen = {}; self.dsems = []; self.dk = 0; self.pend = False


class FW:
    NDMA = 16

    def __init__(self, nc):
        self.nc = nc; self.E = {}
        for n, e in (("pe", nc.tensor), ("act", nc.scalar), ("dve", nc.vector), ("pool", nc.gpsimd), ("sp", nc.sync)):
            self.E[n] = Eng(self, n, e)
        self.allsems = {}

    def _wait(self, e, tok):
        sem, val = tok
        if e.seen.get(sem.name, 0) >= val:
            return
        e.eng.wait_ge(sem, val); e.seen[sem.name] = val

    @staticmethod
    def _ts(bufs):
        return [b.t if isinstance(b, Buf) else b for b in bufs]

    def _deps(self, e, reads, writes):
        best = {}

        def add(tok):
            n = tok[0].name
            if n not in best or best[n][1] < tok[1]:
                best[n] = tok
        for t in reads:
            for tok in t.lw.values():
                add(tok)
        for t in writes:
            if t.rd:
                for tok in list(t.rd.values()) + list(t.lw.values()):
                    if tok[0] is not e.sem:
                        add(tok)
        for tok in best.values():
            self._wait(e, tok)

    def _mark(self, tok, reads, writes):
        n = tok[0].name
        for t in writes:
            if t.rd:
                t.lw = {}; t.rd = {}
            t.lw[n] = tok
        for t in reads:
            t.rd[n] = tok

    def op(self, en, fn, reads=(), writes=(), inc=True):
        e = self.E[en]
        reads = self._ts(reads); writes = self._ts(writes)
        self._deps(e, reads, writes)
        ins = fn(e.eng)
        if inc:
            e.cnt += 1
            ins.then_inc(e.sem, 1)
            tok = (e.sem, e.cnt); e.pend = False
        else:
            tok = (e.sem, e.cnt + 1); e.pend = True
        self._mark(tok, reads, writes)
        return ins

    def dma(self, qn, fn, reads=(), writes=()):
        e = self.E[qn]
        reads = self._ts(reads); writes = self._ts(writes)
        self._deps(e, reads, writes)
        k = e.dk; e.dk += 1
        slot = k % self.NDMA
        if slot >= len(e.dsems):
            e.dsems.append(self.nc.alloc_semaphore("d_%s_%d" % (qn, slot)))
        sem = e.dsems[slot]; val = 16 * (k // self.NDMA + 1)
        if val > 16:
            self._wait(e, (sem, val - 16))
        ins = fn(e.eng)
        ins.then_inc(sem, 16)
        tok = (sem, val)
        self.allsems[sem.name] = tok
        self._mark(tok, reads, writes)
        return tok

    def barrier(self):
        toks = []
        for e in self.E.values():
            assert not e.pend
            if e.cnt:
                toks.append((e.sem, e.cnt))
        toks += list(self.allsems.values())
        for e in self.E.values():
            for tok in toks:
                if tok[0] is not e.sem:
                    self._wait(e, tok)


def build(upto="all", dbg=False):
    nc = bass.Bass("TRN2", target_bir_lowering=False)
    fw = FW(nc)
    outs = {}

    def din(name, shape, dt=F32):
        return nc.dram_tensor(name, list(shape), dt, kind="ExternalInput").ap()

    def dscr(name, shape, dt):
        if dbg:
            outs[name] = (shape, dt)
            return nc.dram_tensor(name, list(shape), dt, kind="ExternalOutput").ap()
        return nc.dram_tensor(name, list(shape), dt).ap()

    xo = din("xo", [NTOK, D]); xp = din("xp", [NTOK, D])
    w_in = din("w_in", [D, IN_COLS])
    g1p = din("g1p", [128, 8]); g2p = din("g2p", [128, 8])
    wa2 = din("wa2", [16, 512]); ba = din("ba", [128, 4]); gog = din("gog", [128, 1])
    dqg = din("dqg", [128, 3]); dkg = din("dkg", [128, 3])
    wpg = din("wpg", [512, D]); wpa = din("wpa", [512, D]); wbg = din("wbg", [D, 2048]); bbp = din("bbp", [128, 16])
    wo = din("wo", [D, D]); wr = din("wr", [D, 36]); brb = din("brb", [128, 36])
    wg_r = din("wg_r", [NEXP * 128, 8 * DEXP]); wu_r = din("wu_r", [NEXP * 128, 8 * DEXP]); wd_r = din("wd_r", [NEXP * 128, 4 * D])
    c_ident = din("c_ident", [128, 128]); c_causal = din("c_causal", [128, 128]); c_ustrict = din("c_ustrict", [128, 128])
    c_iota = din("c_iota", [128, 1]); c_bOh = din("c_bOh", [128, 12 * 256]); c_bOl = din("c_bOl", [128, 12 * 256]); c_bHh = din("c_bHh", [128, 12 * 128]); c_bHl = din("c_bHl", [128, 12 * 128])
    out = nc.dram_tensor("out", [NTOK, D], F32, kind="ExternalOutput").ap()

    GQ = dscr("GQ", [4, 128, NTOK], BF16); GK = dscr("GK", [4, 128, 2 * NTOK], BF16); GR = dscr("GR", [4, 128, NTOK], BF16)
    LA = dscr("LA", [4, 128, 2 * NTOK], F32); GV = dscr("GV", [64, 128, 512], BF16)
    DQ = [dscr("DQ%d" % g, [4, 128, NTOK], BF16) for g in range(3)]
    DK = [dscr("DK%d" % g, [4, 128, 2 * NTOK], BF16) for g in range(3)]
    DV = [dscr("DV%d" % g, [4, 128, 48, 128], BF16) for g in range(3)]
    HT = dscr("HT", [8, 128, NTOK], BF16)
    OG = dscr("OG", [4, 128, NTOK], BF16); OA = dscr("OA", [4, 128, NTOK], BF16)
    X1 = dscr("X1", [NTOK, D], F32); XN = dscr("XN", [NTOK, D], BF16)
    XS = dscr("XS", [PROWS, D], BF16); YB = dscr("YB", [PROWS, D], F32)
    WGS = dscr("WGS", [NEXP * 128, 8 * DEXP], BF16); WUS = dscr("WUS", [NEXP * 128, 8 * DEXP], BF16); WDS = dscr("WDS", [NEXP * 128, 4 * D], BF16)

    cnt = [0]

    def nm(p):
        cnt[0] += 1
        return "%s_%d" % (p, cnt[0])

    def act(o, i, func, R, W, **kw):
        return fw.op("act", lambda e: e.activation(out=o, in_=i, func=func, **kw), R, W)

    def mm(o, lhsT, rhs, st, sp, R, W, inc=None):
        return fw.op("pe", lambda e: e.matmul(o, lhsT=lhsT, rhs=rhs, start=st, stop=sp), R, W, inc=(sp if inc is None else inc))

    def tr(o, i, ident, R, W, inc=True):
        return fw.op("pe", lambda e: e.transpose(out=o, in_=i, identity=ident), R, W, inc=inc)

    def ld(o, i, W, R=()):
        return fw.dma("sp", lambda e: e.dma_start(out=o, in_=i), R, W)

    def st(o, i, R, W=()):
        return fw.dma("pool", lambda e: e.dma_start(out=o, in_=i), R, W)

    def tt(en, o, a, b, op, R, W):
        return fw.op(en, lambda e: e.tensor_tensor(out=o, in0=a, in1=b, op=op), R, W)

    def tsc(en, o, a, s1, s2, op0, op1, R, W, **kw):
        if en == "pool" and op0 == ALU.mult and op1 == ALU.bypass:
            s2 = 0.0; op1 = ALU.add
        return fw.op(en, lambda e: e.tensor_scalar(out=o, in0=a, scalar1=s1, scalar2=s2, op0=op0, op1=op1, **kw), R, W)

    def stt(o, a, s, b, op0, op1, R, W):
        return fw.op("dve", lambda e: e.scalar_tensor_tensor(out=o, in0=a, scalar=s, in1=b, op0=op0, op1=op1), R, W)

    def cp(en, o, i, R, W):
        if en == "act":
            return fw.op(en, lambda e: e.activation(out=o, in_=i, func=AF.Copy), R, W)
        return fw.op(en, lambda e: e.tensor_copy(out=o, in_=i), R, W)

    with ExitStack() as top:
        def SB(stack, name, shape, dt):
            return Buf(stack.enter_context(nc.sbuf_tensor(nm(name), list(shape), dt)))

        def PS(stack, name, shape, dt):
            return Buf(stack.enter_context(nc.psum_tensor(nm(name), list(shape), dt)))

        identf = SB(top, "identf", [128, 128], F32); identb = SB(top, "identb", [128, 128], BF16)
        onesb = SB(top, "onesb", [128, 128], BF16)
        g1s = SB(top, "g1s", [128, 8], F32); g2s = SB(top, "g2s", [128, 8], F32)
        ld(identf[:], c_ident[:, :], [identf]); ld(g1s[:], g1p[:, :], [g1s]); ld(g2s[:], g2p[:, :], [g2s])
        cp("dve", identb[:], identf[:], [identf], [identb])
        fw.op("pool", lambda e: e.memset(onesb[:], 1.0), [], [onesb])
        zt = SB(top, "zt", [128, D], BF16)
        fw.op("pool", lambda e: e.memset(zt[:], 0.0), [], [zt])
        for h_ in range(4):
            st(DV[0][h_, :, 0:8, :], zt[:, 0:1024].rearrange("p (t e) -> p t e", e=128), [zt])
            st(DV[0][h_, :, 8:15, :], zt[:, 0:896].rearrange("p (t e) -> p t e", e=128), [zt])
            for r_ in range(4):
                st(DV[1][h_, :, r_ * 4:r_ * 4 + 3, :], zt[:, 0:384].rearrange("p (t e) -> p t e", e=128), [zt])
        xs_fill = [0]

        def xs_zero(n_):
            for _ in range(n_):
                if xs_fill[0] < NBLK:
                    b_ = xs_fill[0]; xs_fill[0] += 1
                    st(XS[b_ * 128:(b_ + 1) * 128, :], zt[:], [zt])

        with ExitStack() as ph:
            wib = SB(ph, "wib", [128, 8, IN_COLS], BF16)
            stg = [SB(ph, "wstg", [128, 1668], F32) for _ in range(4)]
            w_v = w_in.rearrange("(kc p) n -> p kc n", p=128)
            k = 0
            for kc in range(8):
                for hf in range(4):
                    s = stg[k % 4]
                    ld(s[:], w_v[:, kc, hf * 1668:(hf + 1) * 1668], [s])
                    if k % 4 == 3:
                        act(wib[:, kc, hf * 1668:(hf + 1) * 1668], s[:], AF.Copy, [s, g1s], [wib], scale=g1s[:, kc:kc + 1])
                    else:
                        en = "dve" if k % 2 == 0 else "pool"
                        tsc(en, wib[:, kc, hf * 1668:(hf + 1) * 1668], s[:], g1s[:, kc:kc + 1], None, ALU.mult, ALU.bypass, [s, g1s], [wib])
                    k += 1
            wa2s = SB(ph, "wa2s", [16, 512], F32); bas = SB(ph, "bas", [128, 4], F32); nbas = SB(ph, "nbas", [128, 4], F32)
            dqs = SB(ph, "dqs", [128, 3], F32); dks = SB(ph, "dks", [128, 3], F32); dqss = SB(ph, "dqss", [128, 3], F32)
            ld(wa2s[:], wa2[:, :], [wa2s]); ld(bas[:], ba[:, :], [bas]); ld(dqs[:], dqg[:, :], [dqs]); ld(dks[:], dkg[:, :], [dks])
            tsc("dve", nbas[:], bas[:], -1.0, None, ALU.mult, ALU.bypass, [bas], [nbas])
            tsc("dve", dqss[:], dqs[:], 128.0 ** -0.5, None, ALU.mult, ALU.bypass, [dqs], [dqss])
            hT = SB(ph, "hT", [128, 8, MGS], BF16)
            xt = [SB(ph, "xt", [128, D], F32) for _ in range(3)]
            xn = [SB(ph, "xn", [128, D], BF16) for _ in range(2)]
            junk = SB(ph, "junk", [128, D], BF16)
            ssq = [SB(ph, "ssq", [128, 1], F32) for _ in range(2)]
            lnv = [SB(ph, "lnv", [128, 1], F32) for _ in range(2)]
            rstd = [SB(ph, "rstd", [128, 1], F32) for _ in range(2)]
            ob = [SB(ph, "ob", [128, 512], BF16) for _ in range(4)]
            of = [SB(ph, "of", [128, 512], F32) for _ in range(2)]
            sq = [SB(ph, "sq", [128, 512], BF16) for _ in range(3)]
            t1 = [SB(ph, "t1", [128, 512], F32) for _ in range(2)]
            t2 = [SB(ph, "t2", [128, 512], F32) for _ in range(2)]
            gaf = SB(ph, "gaf", [16, 512], F32)
            ptr = PS(ph, "ptr", [128, 1024], BF16)
            pp = [PS(ph, "pp", [128, 512], F32) for _ in range(4)]
            pst = [PS(ph, "pst", [128, 512], F32) for _ in range(2)]
            pga = PS(ph, "pga", [128, 512], F32)
            rr = {"ob": 0, "of": 0, "pp": 0, "sq": 0, "pst": 0, "x": 0}

            pendq = []

            def nxt(key, lst):
                r = lst[rr[key] % len(lst)]; rr[key] += 1
                return r

            def proj_fm(col0, ncol, tok0):
                p = nxt("pp", pp)
                for kc in range(8):
                    mm(p[0:ncol, :], wib[:, kc, col0:col0 + ncol], hT[:, kc, tok0:tok0 + 512], kc == 0, kc == 7, [wib, hT], [p])
                return p

            def proj_tm(col0, tsl):
                p = nxt("pp", pp)
                for kc in range(8):
                    mm(p[:, :], hT[:, kc, tsl], wib[:, kc, col0:col0 + 512], kc == 0, kc == 7, [wib, hT], [p])
                return p

            def xload(gidx):
                q_ = gidx // 16; tix_ = gidx % 16
                src_ = xo if q_ >= 2 else xp
                base_ = (q_ - 2) * MGS if q_ >= 2 else q_ * MGS
                ld(xt[gidx % 3][:], src_[base_ + tix_ * 128: base_ + (tix_ + 1) * 128, :], [xt[gidx % 3]])

            def stageA(gidx):
                i_ = gidx % 2; x_ = xt[gidx % 3]
                act(junk[:], x_[:], AF.Square, [x_, junk], [junk, ssq[i_]], accum_out=ssq[i_][:])
                act(lnv[i_][:], ssq[i_][:], AF.Ln, [ssq[i_]], [lnv[i_]], scale=1.0 / D, bias=EPS)
                act(rstd[i_][:], lnv[i_][:], AF.Exp, [lnv[i_]], [rstd[i_]], scale=-0.5)
            xload(0); xload(1); stageA(0)
            for q in range(4):
                own = q >= 2
                ext0 = q * MGS
                for tix in range(16):
                    gidx = q * 16 + tix
                    i = gidx % 2
                    if gidx + 2 < 64:
                        xload(gidx + 2)
                    if gidx + 1 < 64:
                        stageA(gidx + 1)
                    x_ = xt[gidx % 3]
                    tsc("dve", xn[i][:], x_[:], rstd[i][:, 0:1], None, ALU.mult, ALU.bypass, [x_, rstd[i]], [xn[i]])
                    for kc in range(8):
                        tr(ptr[:, kc * 128:(kc + 1) * 128], xn[i][:, kc * 128:(kc + 1) * 128], identb[:], [xn[i], identb], [ptr], inc=(kc == 7))
                    cp("act" if tix % 2 else "dve", hT[:, :, tix * 128:(tix + 1) * 128], ptr[:, :].rearrange("p (k t) -> p k t", k=8), [ptr], [hT])
                if own:
                    o0 = (q - 2) * MGS
                    for kc in range(8):
                        st(HT[kc, :, o0:o0 + MGS], hT[:, kc, :], [hT])
                for sg in range(4):
                    tok0 = sg * 512; e0 = ext0 + tok0
                    for h in range(4):
                        p = proj_fm(512 + h * 128, 128, tok0)
                        o = nxt("ob", ob)
                        cp("act", o[:], p[:, :], [p], [o])
                        st(GK[h, :, e0:e0 + 512], o[:], [o])
                    for kc in range(8):
                        mm(pga[0:16, :], wib[:, kc, 2048:2064], hT[:, kc, tok0:tok0 + 512], kc == 0, kc == 7, [wib, hT], [pga])
                    cp("dve", gaf[:], pga[0:16, :], [pga], [gaf])
                    for h in range(4):
                        p = nxt("pp", pp)
                        mm(p[:, :], wa2s[:, h * 128:(h + 1) * 128], gaf[:], True, True, [wa2s, gaf], [p])
                        a = nxt("of", of)
                        act(a[:], p[:, :], AF.Exp, [p, nbas], [a], scale=-1.0, bias=nbas[:, h:h + 1])
                        act(a[:], a[:], AF.Ln, [a], [a], bias=1.0)
                        st(LA[h, :, e0:e0 + 512], a[:], [a])
                for tix in range(16):
                    p = proj_tm(1024, slice(tix * 128, (tix + 1) * 128))
                    o = nxt("ob", ob)
                    cp("act" if tix % 2 else "dve", o[:], p[:, :], [p], [o])
                    st(GV[q * 16 + tix, :, :], o[:], [o])
                if own:
                    o0 = (q - 2) * MGS
                    for sg in range(4):
                        tok0 = sg * 512
                        for h in range(4):
                            p = proj_fm(h * 128, 128, tok0)
                            o = nxt("ob", ob)
                            act(o[:], p[:, :], AF.Copy, [p], [o], scale=128.0 ** -0.5)
                            st(GQ[h, :, o0 + tok0:o0 + tok0 + 512], o[:], [o])
                    for sg in range(4):
                        tok0 = sg * 512
                        for h in range(4):
                            p = proj_fm(1536 + h * 128, 128, tok0)
                            o = nxt("ob", ob)
                            act(o[:], p[:, :], AF.Silu, [p], [o])
                            st(GR[h, :, o0 + tok0:o0 + tok0 + 512], o[:], [o])
                if q >= 1:
                    for g in range(3):
                        d = DILS[g]
                        cbase = 2064 + g * 1536
                        sgs = range(4) if (own or g == 2) else [3]
                        for which in ((0, 1) if own else (1,)):
                            for sg in sgs:
                                tok0 = sg * 512
                                for h in range(4):
                                    p = proj_fm(cbase + which * 512 + h * 128, 128, tok0)
                                    s_ = nxt("sq", sq)
                                    act(s_[:], p[:, :], AF.Square, [p], [s_])

                                    def post(p=p, s_=s_, which=which, g=g, h=h, tok0=tok0):
                                        ps_ = nxt("pst", pst)
                                        mm(ps_[:, :], onesb[:], s_[:], True, True, [onesb, s_], [ps_])
                                        a = t1[rr["pst"] % 2]; b = t2[rr["pst"] % 2]
                                        act(a[:], ps_[:, :], AF.Ln, [ps_], [a], scale=1.0 / 128, bias=EPS)
                                        act(b[:], a[:], AF.Exp, [a], [b], scale=-0.5)
                                        o = nxt("ob", ob)
                                        gsc = (dqss if which == 0 else dks)
                                        stt(o[:], p[:, :], gsc[:, g:g + 1], b[:], ALU.mult, ALU.mult, [p, gsc, b], [o])
                                        if which == 0:
                                            st(DQ[g][h, :, (q - 2) * MGS + tok0:(q - 2) * MGS + tok0 + 512], o[:], [o])
                                        else:
                                            st(DK[g][h, :, ext0 + tok0:ext0 + tok0 + 512], o[:], [o])
                                    if pendq:
                                        pendq.pop(0)()
                                    pendq.append(post)
                        while pendq:
                            pendq.pop(0)()
                        nbm = 16 // d
                        for r in range(d):
                            for ml in range(nbm):
                                if not own and ml != nbm - 1:
                                    continue
                                p = proj_tm(cbase + 1024, ss((ml * 128) * d + r, 128, d))
                                o = nxt("ob", ob)
                                cp("act" if ml % 2 else "dve", o[:], p[:, :], [p], [o])
                                st(DV[g][:, :, (q - 1) * 16 + r * nbm + ml, :].rearrange("h p e -> p h e"), o[:].rearrange("p (h e) -> p h e", h=4), [o])
            fw.barrier()
        if upto == "P":
            return nc, outs

        mid = top.enter_context(ExitStack())
        cst = [SB(mid, "cst", [128, 2048], F32) for _ in range(3)]
        ccb = [SB(mid, "ccb", [128, 2048], BF16) for _ in range(3)]

        def conv_gen():
            chunks = [(e_, src, dst, scaled, hf) for e_ in range(NEXP)
                      for (src, dst, scaled) in ((wg_r, WGS, True), (wu_r, WUS, True), (wd_r, WDS, False)) for hf in range(2)]

            def cld(k_):
                e_, src, dst, scaled, hf = chunks[k_]
                s_ = cst[k_ % 3]
                ld(s_[:], src[e_ * 128:(e_ + 1) * 128, hf * 2048:(hf + 1) * 2048], [s_])
            cld(0)
            for k_ in range(len(chunks)):
                e_, src, dst, scaled, hf = chunks[k_]
                s_ = cst[k_ % 3]; c_ = ccb[k_ % 3]
                if k_ + 1 < len(chunks):
                    cld(k_ + 1)
                for kk in range(4):
                    kc = hf * 4 + kk
                    sl_ = slice(kk * 512, (kk + 1) * 512)
                    tsc("pool", c_[:, sl_], s_[:, sl_], (g2s[:, kc:kc + 1] if scaled else 1.0), None, ALU.mult, ALU.bypass, [s_, g2s], [c_])
                st(dst[e_ * 128:(e_ + 1) * 128, hf * 2048:(hf + 1) * 2048], c_[:], [c_])
                yield
        conv_eng = ["act"]
        conv = conv_gen()

        def conv_step(n_=1):
            for _ in range(n_):
                next(conv, None)

        with ExitStack() as ph:
            gogs = SB(ph, "gogs", [128, 1], F32); causf = SB(ph, "causf", [128, 128], F32)
            caus4 = SB(ph, "caus4", [128, 4, 128], F32)
            rmask = SB(ph, "rmask", [128, 512], F32)
            ld(gogs[:], gog[:, :], [gogs]); ld(causf[:], c_causal[:, :], [causf])
            for h in range(4):
                cp("pool", caus4[:, h, :], causf[:], [causf], [caus4])
            fw.op("pool", lambda e: e.memset(rmask[:], 1.0), [], [rmask])
            fw.op("pool", lambda e: e.memset(rmask[:, ss(0, 4, 128)], 0.0), [rmask], [rmask])
            S = SB(ph, "S", [128, 4, 128], F32); Sb = SB(ph, "Sb", [128, 4, 128], BF16); Dt = SB(ph, "Dt", [128, 4, 128], F32)
            fw.op("pool", lambda e: e.memset(S[:], 0.0), [], [S])
            fw.op("pool", lambda e: e.memset(Sb[:], 0.0), [], [Sb])
            kT = [SB(ph, "kT", [128, 4, 512], BF16) for _ in range(3)]
            la = [SB(ph, "la", [128, 4, 512], F32) for _ in range(3)]
            vv = [SB(ph, "vv", [128, 4, 512], BF16) for _ in range(2)]
            qT = [SB(ph, "qT", [128, 4, 512], BF16) for _ in range(3)]
            grt = [SB(ph, "grt", [128, 4, 512], BF16) for _ in range(2)]
            bc = [SB(ph, "bc", [128, 4, 512], F32) for _ in range(2)]
            en_ = [SB(ph, "en", [128, 4, 512], F32)] * 2; eb_ = [SB(ph, "eb", [128, 4, 512], F32)] * 2
            dec = [SB(ph, "dec", [128, 4, 4], F32) for _ in range(2)]
            kin = [SB(ph, "kin", [128, 4, 512], BF16) for _ in range(2)]; qin = [SB(ph, "qin", [128, 4, 512], BF16) for _ in range(2)]
            kint = [SB(ph, "kint", [128, 512], BF16) for _ in range(4)]
            attm = [SB(ph, "attm", [128, 4, 128], BF16) for _ in range(4)]
            sqo = [SB(ph, "sqo", [128, 512], BF16) for _ in range(2)]
            lno = [SB(ph, "lno", [128, 512], F32) for _ in range(2)]
            rso = [SB(ph, "rso", [128, 512], F32) for _ in range(2)]
            onf = [SB(ph, "onf", [128, 512], F32) for _ in range(2)]
            ogb = [SB(ph, "ogb", [128, 4, 512], BF16) for _ in range(2)]
            pkt = [PS(ph, "pkt", [128, 512], BF16) for _ in range(2)]
            patt = [PS(ph, "patt", [128, 512], F32) for _ in range(2)]
            po = [PS(ph, "po", [128, 512], F32) for _ in range(2)]
            pS = PS(ph, "pS", [128, 512], F32)
            pn = PS(ph, "pn", [128, 512], F32)
            GKv = GK.rearrange("h p t -> p h t"); LAv = LA.rearrange("h p t -> p h t")
            GQv = GQ.rearrange("h p t -> p h t"); GRv = GR.rearrange("h p t -> p h t"); OGv = OG.rearrange("h p t -> p h t")
            ci = 0; pendg = []
            def gloadA(G_):
                i3 = G_ % 3; e0_ = G_ * 512
                ld(kT[i3][:], GKv[:, :, e0_:e0_ + 512], [kT[i3]])
                ld(la[i3][:], LAv[:, :, e0_:e0_ + 512], [la[i3]])
                if G_ >= 8:
                    o0_ = (G_ - 8) * 512
                    ld(qT[i3][:], GQv[:, :, o0_:o0_ + 512], [qT[i3]])

            def gloadB(G_):
                i_ = G_ % 2
                ld(vv[i_][:], GV[G_ * 4:(G_ + 1) * 4, :, :].rearrange("c p n -> p c n"), [vv[i_]])
                if G_ >= 8:
                    o0_ = (G_ - 8) * 512
                    ld(grt[i_][:], GRv[:, :, o0_:o0_ + 512], [grt[i_]])
            def preA(G_, h):
                i_ = G_ % 2; i3 = G_ % 3
                fw.op("dve", lambda e: e.tensor_tensor_scan(out=bc[i_][:, h, :], data0=rmask[:, 0:512], data1=la[i3][:, h, :], initial=0.0, op0=ALU.mult, op1=ALU.add), [rmask, la[i3]], [bc[i_]])
                act(en_[i_][:, h, :], bc[i_][:, h, :], AF.Exp, [bc[i_]], [en_[i_]], scale=1.0 / 16)
                act(dec[i_][:, h, :], bc[i_][:, h, :].rearrange("p (c t) -> p c t", c=4)[:, :, 127], AF.Exp, [bc[i_]], [dec[i_]], scale=-1.0 / 16)
                tt("dve", kin[i_][:, h, :], kT[i3][:, h, :], en_[i_][:, h, :], ALU.mult, [kT[i3], en_[i_]], [kin[i_]])
                if G_ >= 8:
                    act(eb_[i_][:, h, :], bc[i_][:, h, :], AF.Exp, [bc[i_]], [eb_[i_]], scale=-1.0 / 16)
                    tt("pool", qin[i_][:, h, :], qT[i3][:, h, :], eb_[i_][:, h, :], ALU.mult, [qT[i3], eb_[i_]], [qin[i_]])
            gloadA(0); gloadB(0); gloadA(1)
            for h in range(4):
                preA(0, h)
            for G in range(16):
                own = G >= 8
                i = G % 2
                e0 = G * 512
                if G + 2 < 16:
                    gloadA(G + 2)
                if G + 1 < 16:
                    gloadB(G + 1)
                if own:
                    o0 = (G - 8) * 512
                for c in range(4):
                    cs = slice(c * 128, (c + 1) * 128)
                    for h in range(4):
                        tr(pkt[c % 2][:, h * 128:(h + 1) * 128], kin[i][:, h, cs], identb[:], [kin[i], identb], [pkt[c % 2]], inc=(h == 3))
                    cp("act", kint[c][:], pkt[c % 2][:, :], [pkt[c % 2]], [kint[c]])
                    if own:
                        for h in range(4):
                            mm(patt[c % 2][:, h * 128:(h + 1) * 128], kin[i][:, h, cs], qin[i][:, h, cs], True, True, [kin[i], qin[i]], [patt[c % 2]], inc=(h == 3))
                        tt("dve", attm[c][:], patt[c % 2][:, :].rearrange("p (h t) -> p h t", h=4), caus4[:], ALU.mult, [patt[c % 2], caus4], [attm[c]])
                for c in range(4):
                    cs = slice(c * 128, (c + 1) * 128)
                    j = ci % 2; ci += 1
                    if own:
                        for h in range(4):
                            mm(po[j][:, h * 128:(h + 1) * 128], vv[i][:, c, h * 128:(h + 1) * 128], attm[c][:, h, :], True, False, [vv[i], attm[c]], [po[j]], inc=False)
                            mm(po[j][:, h * 128:(h + 1) * 128], Sb[:, h, :], qin[i][:, h, cs], False, True, [Sb, qin[i]], [po[j]], inc=(h == 3))
                    for h in range(4):
                        mm(pS[:, h * 128:(h + 1) * 128], kint[c][:, h * 128:(h + 1) * 128], vv[i][:, c, h * 128:(h + 1) * 128], True, True, [kint[c], vv[i]], [pS], inc=(h == 3))
                    tt("dve", Dt[:], pS[:, :].rearrange("p (h t) -> p h t", h=4), S[:], ALU.add, [pS, S], [Dt])
                    decb = dec[i][:, :, c:c + 1].broadcast_to([128, 4, 128])
                    for h in range(4):
                        act(Sb[:, h, :], Dt[:, h, :], AF.Copy, [Dt, dec[i], Sb], [Sb], scale=dec[i][:, h, c:c + 1])
                    conv_step()
                    xs_zero(2)
                    tt("dve", S[:], Dt[:], decb, ALU.mult, [Dt, dec[i]], [S])
                    if G + 1 < 16:
                        preA(G + 1, c)
                    if own:
                        act(sqo[j][:], po[j][:, :], AF.Square, [po[j]], [sqo[j]])

                        def gpost(j=j, i=i, c=c, cs=cs, o0=o0):
                            mm(pn[:, :], onesb[:], sqo[j][:], True, True, [onesb, sqo[j]], [pn])
                            act(lno[j][:], pn[:, :], AF.Ln, [pn], [lno[j]], scale=1.0 / 128, bias=EPS)
                            act(rso[j][:], lno[j][:], AF.Exp, [lno[j]], [rso[j]], scale=-0.5)
                            stt(onf[j][:], po[j][:, :], gogs[:, 0:1], rso[j][:], ALU.mult, ALU.mult, [po[j], gogs, rso[j]], [onf[j]])
                            tt("pool", ogb[i][:, :, cs], onf[j][:].rearrange("p (h t) -> p h t", h=4), grt[i][:, :, cs], ALU.mult, [onf[j], grt[i]], [ogb[i]])
                            if c == 3:
                                st(OGv[:, :, o0:o0 + 512], ogb[i][:], [ogb[i]])
                        if pendg:
                            pendg.pop(0)()
                        pendg.append(gpost)
                while pendg:
                    pendg.pop(0)()
            while pendg:
                pendg.pop(0)()
            fw.barrier()
        if upto == "G":
            return nc, outs

        with ExitStack() as ph:
            bOf = SB(ph, "bOf", [128, 12 * 256], F32); bHf = SB(ph, "bHf", [128, 12 * 128], F32)
            bOh = SB(ph, "bOh", [128, 12, 256], BF16); bOl = SB(ph, "bOl", [128, 12, 256], BF16)
            bHh = SB(ph, "bHh", [128, 12, 128], BF16); bHl = SB(ph, "bHl", [128, 12, 128], BF16)
            for (srcO, srcH, dO, dH) in ((c_bOh, c_bHh, bOh, bHh), (c_bOl, c_bHl, bOl, bHl)):
                ld(bOf[:], srcO[:, :], [bOf]); ld(bHf[:], srcH[:, :], [bHf])
                cp("dve", dO[:].rearrange("p a b -> p (a b)"), bOf[:], [bOf], [dO])
                cp("dve", dH[:].rearrange("p a b -> p (a b)"), bHf[:], [bHf], [dH])
            acc = SB(ph, "acc", [128, 2, NTOK], F32)
            kTd = [SB(ph, "kTd", [128, 2048 + NTOK], BF16) for _ in range(2)]
            qTd = [SB(ph, "qTd", [128, NTOK], BF16) for _ in range(2)]
            va = [SB(ph, "va", [128, 48, 128], BF16) for _ in range(2)]
            pm = [SB(ph, "pm", [128, 256], BF16) for _ in range(4)]
            rden = SB(ph, "rden", [128, NTOK], F32)
            oab = SB(ph, "oab", [128, NTOK], BF16)
            psc = [PS(ph, "psc", [128, 256], F32) for _ in range(4)]
            pov = [PS(ph, "pov", [128, 256], F32) for _ in range(3)]
            gi = 0; ri = 0; ki = 0; oi = 0
            hg_list = [(h_, g_) for h_ in range(4) for g_ in range(3)]
            hgr_list = [(h_, g_, r_) for (h_, g_) in hg_list for r_ in range(DILS[g_])]

            def kqload(ix):
                h_, g_ = hg_list[ix]; hb_ = 128 * DILS[g_]
                ld(kTd[ix % 2][:, 0:hb_ + NTOK], DK[g_][h_, :, NTOK - hb_:2 * NTOK], [kTd[ix % 2]])
                ld(qTd[ix % 2][:], DQ[g_][h_, :, :], [qTd[ix % 2]])
                ld(va[ix % 2][:], DV[g_][h_, :, :, :], [va[ix % 2]])
            kqload(0)
            conv_eng[0] = "pool"
            for h in range(4):
                for g in range(3):
                    d = DILS[g]; hb = 128 * d; nb = 32 // d; nbm = 16 // d
                    kt = kTd[gi % 2]; qt = qTd[gi % 2]; v = va[gi % 2]; gi += 1
                    if gi < len(hg_list):
                        kqload(gi)
                    mi = g * 4 + h
                    for r in range(d):
                        pms = {}

                        def vix(m, r=r, nbm=nbm):
                            if m < 0:
                                return r * nbm + nbm - 1
                            return 16 * (1 + m // nbm) + r * nbm + (m % nbm)

                        def QK(m):
                            nonlocal ki
                            if m < 0:
                                ks = ss(r, 128, d); qs = ss(r, 128, d); N = 128
                                bh = bHh[:, mi, :]; bl = bHl[:, mi, :]; bb_ = (bHh, bHl)
                            else:
                                ks = ss(hb + (m * 128) * d + r, 128, d)
                                N = 256 if m < nb - 1 else 128
                                qs = ss((m * 128) * d + r, N, d)
                                bh = bOh[:, mi, 0:N]; bl = bOl[:, mi, 0:N]; bb_ = (bOh, bOl)
                            p = psc[ki % 4]; pmb = pm[ki % 4]
                            if ki % 7 == 0:
                                conv_step()
                            ki += 1
                            mm(p[:, 0:N], kt[:, ks], qt[:, qs], True, False, [kt, qt], [p], inc=False)
                            mm(p[:, 0:N], identb[:], bh, False, False, [identb, bb_[0]], [p], inc=False)
                            mm(p[:, 0:N], identb[:], bl, False, True, [identb, bb_[1]], [p], inc=True)
                            act(pmb[:, 0:N], p[:, 0:N], AF.Exp, [p], [pmb])
                            pms[m] = pmb

                        def PV(m):
                            nonlocal oi
                            po_ = pov[oi % 3]; oi += 1
                            pprev = pms[m - 1]; pmb = pms[m]
                            rp = pprev[:, 0:128] if m == 0 else pprev[:, 128:256]
                            mm(po_[:, 0:128], v[:, vix(m - 1), :], rp, True, False, [v, pprev], [po_], inc=False)
                            mm(po_[:, 0:128], v[:, vix(m), :], pmb[:, 0:128], False, True, [v, pmb], [po_], inc=False)
                            mm(po_[:, 128:256], onesb[:], rp, True, False, [onesb, pprev], [po_], inc=False)
                            mm(po_[:, 128:256], onesb[:], pmb[:, 0:128], False, True, [onesb, pmb], [po_], inc=True)
                            av = acc[:, :, ss((m * 128) * d + r, 128, d)]
                            if g == 0:
                                cp("dve", av, po_[:, :].rearrange("p (a t) -> p a t", a=2), [po_], [acc])
                            else:
                                tt("dve", av, po_[:, :].rearrange("p (a t) -> p a t", a=2), av, ALU.add, [po_, acc], [acc])
                        blocks = list(range(-1, nb))
                        QK(blocks[0])
                        for bi, m in enumerate(blocks):
                            if bi + 1 < len(blocks):
                                QK(blocks[bi + 1])
                            if m >= 0:
                                PV(m)
                act(rden[:], acc[:, 1, :], AF.Ln, [acc], [rden])
                act(rden[:], rden[:], AF.Exp, [rden], [rden], scale=-1.0)
                tt("dve", oab[:], acc[:, 0, :], rden[:], ALU.mult, [acc, rden], [oab])
                st(OA[h, :, :], oab[:], [oab])
            conv_eng[0] = "act"
            fw.barrier()
        if upto == "D":
            return nc, outs

        OH1 = SB(top, "OH1", [128, 32, 32], F32); OH2 = SB(top, "OH2", [128, 32, 32], F32)
        G1 = SB(top, "G1", [128, 32], F32); G2 = SB(top, "G2", [128, 32], F32)
        LG = SB(top, "LG", [128, 32, 36], F32)
        with ExitStack() as ph:
            wpgb = SB(ph, "wpgb", [128, 4, D], BF16); wpab = SB(ph, "wpab", [128, 4, D], BF16)
            wbb = SB(ph, "wbb", [128, 8, 2048], BF16); wob = SB(ph, "wob", [128, 8, D], BF16)
            wrs = SB(ph, "wrs", [128, 8, 36], F32); brs = SB(ph, "brs", [128, 36], F32); bbs = SB(ph, "bbs", [128, 16], F32)
            ld(brs[:], brb[:, :], [brs]); ld(bbs[:], bbp[:, :], [bbs])
            with ExitStack() as wsc:
                stg = [SB(wsc, "mstg", [128, 2048], F32) for _ in range(2)]
                k = 0

                def wload(dst, src, rows_k, ncols, scale):
                    nonlocal k
                    v_ = src.rearrange("(kc p) n -> p kc n", p=128)
                    for kc in range(rows_k):
                        s = stg[k % 2]; k += 1
                        ld(s[:, 0:ncols], v_[:, kc, :], [s])
                        if k % 2 == 0:
                            if scale is None:
                                cp("dve", dst[:, kc, :], s[:, 0:ncols], [s], [dst])
                            else:
                                tsc("dve", dst[:, kc, :], s[:, 0:ncols], scale[:, kc:kc + 1], None, ALU.mult, ALU.bypass, [s, scale], [dst])
                        else:
                            if scale is None:
                                cp("act", dst[:, kc, :], s[:, 0:ncols], [s], [dst])
                            else:
                                act(dst[:, kc, :], s[:, 0:ncols], AF.Copy, [s, scale], [dst], scale=scale[:, kc:kc + 1])
                wload(wpgb, wpg, 4, D, None); wload(wpab, wpa, 4, D, None)
                wload(wbb, wbg, 8, 2048, g1s); wload(wob, wo, 8, D, None)
                wload(wrs, wr, 8, 36, g2s)
            fw.barrier()
            og = [SB(ph, "og", [128, 4, 512], BF16) for _ in range(2)]
            oa = [SB(ph, "oa", [128, 4, 512], BF16) for _ in range(2)]
            hTm = [SB(ph, "hTm", [128, 8, 512], BF16) for _ in range(2)]
            xr = [SB(ph, "xr", [128, D], F32) for _ in range(2)]
            sgt = [SB(ph, "sgt", [128, 512], F32) for _ in range(2)]
            sat = [SB(ph, "sat", [128, 512], F32) for _ in range(2)]
            m1t = [SB(ph, "m1t", [128, 512], F32)] * 2
            m2t = [SB(ph, "m2t", [128, 512], F32)] * 2
            zT = SB(ph, "zT", [128, 8, 512], BF16)
            x1t = [SB(ph, "x1t", [128, D], F32) for _ in range(2)]
            xn2 = [SB(ph, "xn2", [128, D], F32) for _ in range(2)]
            xn2b = [SB(ph, "xn2b", [128, D], BF16) for _ in range(2)]
            junk = SB(ph, "junkm", [128, D], BF16)
            xT2 = [SB(ph, "xT2", [128, 8, 128], F32) for _ in range(2)]
            sm = {k_: [SB(ph, k_, [128, w_], F32) for _ in range(2)] for k_, w_ in
                  (("ss2", 1), ("ln2", 1), ("rs2", 1))}
            pM = [PS(ph, "pM", [128, 512], F32) for _ in range(8)]
            OGv = OG.rearrange("h p t -> p h t"); OAv = OA.rearrange("h p t -> p h t"); HTv = HT.rearrange("k p t -> p k t")
            ti = 0; rq = []; rq2 = []
            def mload(Gp_):
                i_ = Gp_ % 2; o0_ = Gp_ * 512
                ld(og[i_][:], OGv[:, :, o0_:o0_ + 512], [og[i_]]); ld(oa[i_][:], OAv[:, :, o0_:o0_ + 512], [oa[i_]])
                ld(hTm[i_][:], HTv[:, :, o0_:o0_ + 512], [hTm[i_]])

            def xrload(t__):
                ld(xr[t__ % 2][:], xo[t__ * 128:(t__ + 1) * 128, :], [xr[t__ % 2]])
            mload(0); xrload(0)
            for Gp in range(8):
                i = Gp % 2; o0 = Gp * 512
                if Gp + 1 < 8:
                    mload(Gp + 1)
                for ct in range(8):
                    cs = slice(ct * 128, (ct + 1) * 128)
                    j = ct % 2
                    pA = [pM[4 * j]]; pB = [pM[4 * j + 1]]; pC = [pM[4 * j + 2]]; pD = [pM[4 * j + 3]]
                    conv_step()
                    for kc in range(4):
                        mm(pA[0][:, :], wpgb[:, kc, cs], og[i][:, kc, :], kc == 0, kc == 3, [wpgb, og[i]], [pA[0]])
                    for kc in range(4):
                        mm(pB[0][:, :], wpab[:, kc, cs], oa[i][:, kc, :], kc == 0, kc == 3, [wpab, oa[i]], [pB[0]])
                    for kc in range(8):
                        mm(pC[0][:, :], wbb[:, kc, cs], hTm[i][:, kc, :], kc == 0, kc == 7, [wbb, hTm[i]], [pC[0]])
                    for kc in range(8):
                        mm(pD[0][:, :], wbb[:, kc, D + ct * 128:D + (ct + 1) * 128], hTm[i][:, kc, :], kc == 0, kc == 7, [wbb, hTm[i]], [pD[0]])
                    act(sgt[j][:], pC[0][:, :], AF.Sigmoid, [pC[0], bbs], [sgt[j]], bias=bbs[:, ct:ct + 1])
                    act(sat[j][:], pD[0][:, :], AF.Sigmoid, [pD[0], bbs], [sat[j]], bias=bbs[:, 8 + ct:9 + ct])
                    tt("dve", m1t[j][:], pA[0][:, :], sgt[j][:], ALU.mult, [pA[0], sgt[j]], [m1t[j]])
                    tt("dve", m2t[j][:], pB[0][:, :], sat[j][:], ALU.mult, [pB[0], sat[j]], [m2t[j]])
                    tt("dve", zT[:, ct, :], m1t[j][:], m2t[j][:], ALU.add, [m1t[j], m2t[j]], [zT])
                for c in range(4):
                    t_ = Gp * 4 + c
                    j = ti % 2; ti += 1
                    pX = [pM[4 * j], pM[4 * j + 1]]; pT = pM[4 * j + 2]; pL = pM[4 * j + 3]
                    if t_ + 1 < 32:
                        xrload(t_ + 1)
                    for hf in range(2):
                        for kc in range(8):
                            mm(pX[hf][:, :], zT[:, kc, c * 128:(c + 1) * 128], wob[:, kc, hf * 512:(hf + 1) * 512], kc == 0, kc == 7, [zT, wob], [pX[hf]])
                        tt("dve", x1t[j][:, hf * 512:(hf + 1) * 512], pX[hf][:, :], xr[j][:, hf * 512:(hf + 1) * 512], ALU.add, [pX[hf], xr[j]], [x1t[j]])
                    st(X1[t_ * 128:(t_ + 1) * 128, :], x1t[j][:], [x1t[j]])
                    S_ = {k_: v_[j] for k_, v_ in sm.items()}
                    act(junk[:], x1t[j][:], AF.Square, [x1t[j], junk], [junk, S_["ss2"]], accum_out=S_["ss2"][:])
                    act(S_["ln2"][:], S_["ss2"][:], AF.Ln, [S_["ss2"]], [S_["ln2"]], scale=1.0 / D, bias=EPS)
                    act(S_["rs2"][:], S_["ln2"][:], AF.Exp, [S_["ln2"]], [S_["rs2"]], scale=-0.5)
                    tsc("dve", xn2[j][:], x1t[j][:], S_["rs2"][:, 0:1], None, ALU.mult, ALU.bypass, [x1t[j], S_["rs2"]], [xn2[j]])
                    tsc("pool", xn2b[j][:], xn2[j][:], 1.0, None, ALU.mult, ALU.bypass, [xn2[j]], [xn2b[j]])
                    st(XN[t_ * 128:(t_ + 1) * 128, :], xn2b[j][:], [xn2b[j]])
                    def router1(j=j, t_=t_, S_=S_, pT=pT, pL=pL):
                        for half, pb in ((0, pT), (1, pL)):
                            for kk in range(4):
                                kc = half * 4 + kk
                                tr(pb[:, kk * 128:(kk + 1) * 128], xn2[j][:, kc * 128:(kc + 1) * 128], identf[:], [xn2[j], identf], [pb], inc=(kk == 3))
                        for half, pb in ((0, pT), (1, pL)):
                            cp("act", xT2[j][:, half * 4:(half + 1) * 4, :], pb[:, :].rearrange("p (k t) -> p k t", k=4), [pb], [xT2[j]])
                        rq2.append(lambda: router2(j, t_, S_, pT))

                    def router2(j, t_, S_, pL):
                        for kc in range(8):
                            mm(pL[:, 0:36], xT2[j][:, kc, :], wrs[:, kc, :], kc == 0, kc == 7, [xT2[j], wrs], [pL])
                        tt("dve", LG[:, t_, :], pL[:, 0:36], brs[:], ALU.add, [pL, brs], [LG])

                    r2_ = rq2.pop(0) if rq2 else None
                    if rq:
                        rq.pop(0)()
                    if r2_ is not None:
                        r2_()
                    rq.append(router1)
            while rq or rq2:
                if rq2:
                    rq2.pop(0)()
                if rq:
                    rq.pop(0)()
            fw.barrier()
        if upto == "M":
            return nc, outs

        with ExitStack() as ph:
            conv_step(1000)
            fw.barrier()
            rsc = ExitStack()

            def RB(name, shape):
                return SB(rsc, name, shape, F32)
            gmax = RB("gmax", [128, 32]); ohg = RB("ohg", [128, 32, 4]); dlt = RB("dlt", [128, 32, 4]); exg = RB("exg", [128, 32, 4])
            sumex = RB("sumex", [128, 32]); gp = RB("gp", [128, 32]); pen = RB("pen", [128, 32, 4])
            msk = RB("msk", [128, 32, 32]); msk2 = RB("msk2", [128, 32, 32]); m1 = RB("m1", [128, 32]); m2 = RB("m2", [128, 32])
            dm = RB("dm", [128, 32]); e2 = RB("e2", [128, 32]); dn = RB("dn", [128, 32]); w1 = RB("w1", [128, 32]); w2 = RB("w2", [128, 32])
            glog = LG[:, :, 0:4]; elog = LG[:, :, 4:36]
            fw.op("dve", lambda e: e.reduce_max(out=gmax[:], in_=glog, axis=AX.X), [LG], [gmax])
            gmax_b = gmax[:].unsqueeze(2).broadcast_to([128, 32, 4])
            tt("dve", ohg[:], glog, gmax_b, ALU.is_ge, [LG, gmax], [ohg])
            tt("dve", dlt[:], glog, gmax_b, ALU.subtract, [LG, gmax], [dlt])
            act(exg[:], dlt[:], AF.Exp, [dlt], [exg])
            fw.op("dve", lambda e: e.reduce_sum(out=sumex[:], in_=exg[:], axis=AX.X), [exg], [sumex])
            fw.op("dve", lambda e: e.reciprocal(out=gp[:], in_=sumex[:]), [sumex], [gp])
            tsc("dve", pen[:], ohg[:], -1.0, 1e30, ALU.add, ALU.mult, [ohg], [pen])
            tt("dve", msk[:].rearrange("p t (g j) -> p t g j", g=4), elog.rearrange("p t (g j) -> p t g j", g=4), pen[:].unsqueeze(3).broadcast_to([128, 32, 4, 8]), ALU.add, [LG, pen], [msk])
            fw.op("dve", lambda e: e.reduce_max(out=m1[:], in_=msk[:], axis=AX.X), [msk], [m1])
            tt("dve", OH1[:], msk[:], m1[:].unsqueeze(2).broadcast_to([128, 32, 32]), ALU.is_ge, [msk, m1], [OH1])
            stt(msk2[:].rearrange("p t e -> p (t e)"), OH1[:].rearrange("p t e -> p (t e)"), -1e30, msk[:].rearrange("p t e -> p (t e)"), ALU.mult, ALU.add, [OH1, msk], [msk2])
            fw.op("dve", lambda e: e.reduce_max(out=m2[:], in_=msk2[:], axis=AX.X), [msk2], [m2])
            tt("dve", OH2[:], msk2[:], m2[:].unsqueeze(2).broadcast_to([128, 32, 32]), ALU.is_ge, [msk2, m2], [OH2])
            tt("dve", dm[:], m2[:], m1[:], ALU.subtract, [m1, m2], [dm])
            act(e2[:], dm[:], AF.Exp, [dm], [e2])
            tsc("dve", dn[:], e2[:], 1.0, None, ALU.add, ALU.bypass, [e2], [dn])
            fw.op("dve", lambda e: e.reciprocal(out=w1[:], in_=dn[:]), [dn], [w1])
            tt("dve", w2[:], w1[:], e2[:], ALU.mult, [w1, e2], [w2])
            tt("dve", G1[:], w1[:], gp[:], ALU.mult, [w1, gp], [G1])
            tt("dve", G2[:], w2[:], gp[:], ALU.mult, [w2, gp], [G2])
            fw.barrier()
            rsc.close()
            ustf = SB(ph, "ustf", [128, 128], F32); ustb = SB(ph, "ustb", [128, 128], BF16); iotas = SB(ph, "iotas", [128, 1], F32)
            ld(ustf[:], c_ustrict[:, :], [ustf]); ld(iotas[:], c_iota[:, :], [iotas])
            cp("dve", ustb[:], ustf[:], [ustf], [ustb])
            oh12 = SB(ph, "oh12", [128, 32, 32], BF16)
            tt("dve", oh12[:], OH1[:], OH2[:], ALU.add, [OH1, OH2], [oh12])
            bk = ExitStack()
            pex = [PS(bk, "pex", [128, 512], F32) for _ in range(2)]
            pcn = PS(bk, "pcn", [128, 512], F32)
            for t_ in range(32):
                pt = pex[t_ // 16]; sl = slice((t_ % 16) * 32, (t_ % 16 + 1) * 32)
                mm(pt[:, sl], ustb[:], oh12[:, t_, :], True, t_ == 0, [ustb, oh12], [pt], inc=(t_ == 0))
                for t2_ in range(t_):
                    mm(pt[:, sl], onesb[:], oh12[:, t2_, :], False, t2_ == t_ - 1, [onesb, oh12], [pt], inc=(t2_ == t_ - 1))
            for t_ in range(32):
                mm(pcn[:, 0:32], onesb[:], oh12[:, t_, :], t_ == 0, t_ == 31, [onesb, oh12], [pcn])
            cntf = SB(ph, "cntf", [128, 32], F32); cnti = SB(ph, "cnti", [128, 32], I32); pcf = SB(ph, "pcf", [128, 32], F32)
            pend = SB(ph, "pend", [128, 32], F32); pstart = SB(ph, "pstart", [128, 32], F32); ones32 = SB(ph, "ones32", [128, 32], F32)
            tsc("dve", cntf[:], pcn[:, 0:32], 127.0, None, ALU.add, ALU.bypass, [pcn], [cntf])
            cp("dve", cnti[:], cntf[:], [cntf], [cnti])
            tsc("dve", cnti[:], cnti[:], 7, 7, ALU.arith_shift_right, ALU.logical_shift_left, [cnti], [cnti])
            cp("dve", pcf[:], cnti[:], [cnti], [pcf])
            fw.op("pool", lambda e: e.memset(ones32[:], 1.0), [], [ones32])
            fw.op("dve", lambda e: e.tensor_tensor_scan(out=pend[:], data0=ones32[:], data1=pcf[:], initial=0.0, op0=ALU.mult, op1=ALU.add), [ones32, pcf], [pend])
            tt("dve", pstart[:], pend[:], pcf[:], ALU.subtract, [pend, pcf], [pstart])
            base = SB(ph, "base", [128, 32, 32], F32); prod = SB(ph, "prod", [128, 32, 32], F32)
            d1f = SB(ph, "d1f", [128, 32], F32); d2f = SB(ph, "d2f", [128, 32], F32)
            d1i = SB(ph, "d1i", [128, 32], I32); d2i = SB(ph, "d2i", [128, 32], I32)
            for t_ in range(32):
                pt = pex[t_ // 16]; sl = slice((t_ % 16) * 32, (t_ % 16 + 1) * 32)
                tt("dve", base[:, t_, :], pt[:, sl], pstart[:], ALU.add, [pt, pstart], [base])
            for (ohx, dxf, dxi) in ((OH1, d1f, d1i), (OH2, d2f, d2i)):
                tt("dve", prod[:], ohx[:], base[:], ALU.mult, [ohx, base], [prod])
                fw.op("dve", lambda e: e.reduce_sum(out=dxf[:], in_=prod[:], axis=AX.X), [prod], [dxf])
                cp("dve", dxi[:], dxf[:], [dxf], [dxi])
            xs_t = T()
            xg = [SB(ph, "xg", [128, D], BF16) for _ in range(2)]
            for t_ in range(32):
                b_ = xg[t_ % 2]
                ld(b_[:], XN[t_ * 128:(t_ + 1) * 128, :], [b_])
                for dxi in (d1i, d2i):
                    fw.dma("pool", lambda e: e.indirect_dma_start(out=XS[:, :], out_offset=bass.IndirectOffsetOnAxis(ap=dxi[:, t_:t_ + 1], axis=0), in_=b_[:], in_offset=None), [b_, dxi], [xs_t])
            bef = SB(ph, "bef", [128, NBLK], F32); bjunk = SB(ph, "bjunk", [128, 32], F32); idxw = SB(ph, "idxw", [128, NBLK], I32)
            for n in range(NBLK):
                tsc("dve", bjunk[:], pend[:], float(n * 128) + 0.5, None, ALU.is_le, ALU.add, [pend, bjunk], [bjunk, bef], accum_out=bef[:, n:n + 1])
            sf = SB(ph, "sf", [128, NBLK], F32)
            tsc("dve", bef[:], bef[:], 31.0, None, ALU.min, ALU.bypass, [bef], [bef])
            fw.op("dve", lambda e: e.memset(sf[:], 0.0), [], [sf])
            tt("dve", sf[:, 1:NBLK], bef[:, 1:NBLK], bef[:, 0:NBLK - 1], ALU.is_equal, [bef], [sf])
            for bnd in (NBLK // 3, 2 * NBLK // 3):
                fw.op("dve", lambda e: e.memset(sf[:, bnd:bnd + 1], 0.0), [sf], [sf])
            tsc("dve", bef[:], bef[:], 128.0, iotas[:, 0:1], ALU.mult, ALU.add, [bef, iotas], [bef])
            stt(bef[:], sf[:], 1.0e6, bef[:], ALU.mult, ALU.add, [sf, bef], [bef])
            cp("dve", idxw[:], bef[:], [bef], [idxw])
            fw.barrier()
            bk.close()
            NL = 3; LB = NBLK // NL
            xin = [SB(ph, "xin", [128, D], BF16) for _ in range(4)]
            xinT = [SB(ph, "xinT", [128, 8, 128], BF16) for _ in range(2)]
            wgs = [SB(ph, "wgs", [128, 8, DEXP], BF16) for _ in range(NL)]
            wus = [SB(ph, "wus", [128, 8, DEXP], BF16) for _ in range(NL)]
            wds = [SB(ph, "wds", [128, 4, D], BF16) for _ in range(NL)]
            sgf = [SB(ph, "sgf", [128, DEXP], F32) for _ in range(2)]
            ab = [SB(ph, "ab", [128, DEXP], BF16) for _ in range(2)]
            aT = [SB(ph, "aT", [128, 4, 128], BF16) for _ in range(2)]
            ybs = [SB(ph, "ybs", [128, D], F32) for _ in range(2)]
            pxt = PS(ph, "pxt", [128, 1024], BF16)
            pg = PS(ph, "pg", [128, 512], F32); pu = PS(ph, "pu", [128, 512], F32)
            pat = PS(ph, "pat", [128, 512], BF16)
            py = [PS(ph, "py", [128, 512], F32) for _ in range(2)]
            bcreg = nc.gpsimd.to_reg(NEXP * 128 - 1)

            def blk(s_):
                return (s_ // NL) + LB * (s_ % NL)

            def SX(s_):
                n = blk(s_)
                ld(xin[s_ % 4][:], XS[n * 128:(n + 1) * 128, :], [xin[s_ % 4]])

            def SW(s_):
                n = blk(s_); i = s_ % NL
                for (wsb, wsrc) in ((wgs[i], WGS), (wus[i], WUS), (wds[i], WDS)):
                    fw.dma("pool", lambda e: e.indirect_dma_start(out=wsb[:].rearrange("p a b -> p (a b)"), out_offset=None, in_=wsrc[:, :], in_offset=bass.IndirectOffsetOnAxis(ap=idxw[:, n:n + 1], axis=0), bounds_check=bcreg, oob_is_err=False), [idxw], [wsb])

            def S1(s_):
                x_ = xin[s_ % 4]
                for kc in range(8):
                    tr(pxt[:, kc * 128:(kc + 1) * 128], x_[:, kc * 128:(kc + 1) * 128], identb[:], [x_, identb], [pxt], inc=(kc == 7))
                cp("dve", xinT[s_ % 2][:], pxt[:, :].rearrange("p (k t) -> p k t", k=8), [pxt], [xinT[s_ % 2]])

            def S2(s_):
                i = s_ % NL; j = s_ % 2
                for kc in range(8):
                    mm(pg[:, :], xinT[j][:, kc, :], wgs[i][:, kc, :], kc == 0, kc == 7, [xinT[j], wgs[i]], [pg])
                for kc in range(8):
                    mm(pu[:, :], xinT[j][:, kc, :], wus[i][:, kc, :], kc == 0, kc == 7, [xinT[j], wus[i]], [pu])
                act(sgf[j][:], pg[:, :], AF.Silu, [pg], [sgf[j]])
                tt("dve", ab[j][:], sgf[j][:], pu[:, :], ALU.mult, [sgf[j], pu], [ab[j]])

            def S3(s_):
                j = s_ % 2
                for fc in range(4):
                    tr(pat[:, fc * 128:(fc + 1) * 128], ab[j][:, fc * 128:(fc + 1) * 128], identb[:], [ab[j], identb], [pat], inc=(fc == 3))
                cp("dve", aT[j][:], pat[:, :].rearrange("p (k t) -> p k t", k=4), [pat], [aT[j]])

            def S4(s_):
                i = s_ % NL; j = s_ % 2; n = blk(s_)
                for hf in range(2):
                    for fc in range(4):
                        mm(py[hf][:, :], aT[j][:, fc, :], wds[i][:, fc, hf * 512:(hf + 1) * 512], fc == 0, fc == 3, [aT[j], wds[i]], [py[hf]])
                    cp("act", ybs[j][:, hf * 512:(hf + 1) * 512], py[hf][:, :], [py[hf]], [ybs[j]])
                st(YB[n * 128:(n + 1) * 128, :], ybs[j][:], [ybs[j]])
            for s_ in range(3):
                SX(s_); SW(s_)
            S1(0); S1(1); S2(0)
            for k_ in range(NBLK):
                S3(k_)
                if k_ + 1 < NBLK:
                    S2(k_ + 1)
                S4(k_)
                if k_ + 2 < NBLK:
                    S1(k_ + 2)
                if k_ + 3 < NBLK:
                    SW(k_ + 3); SX(k_ + 3)
            fw.barrier()
            y1 = [SB(ph, "y1", [128, D], F32) for _ in range(3)]
            y2 = [SB(ph, "y2", [128, D], F32) for _ in range(3)]
            x1r = [SB(ph, "x1r", [128, D], F32) for _ in range(3)]
            def cload(t__):
                i_ = t__ % 3
                ld(x1r[i_][:], X1[t__ * 128:(t__ + 1) * 128, :], [x1r[i_]])
                for (yb_, dxi) in ((y1[i_], d1i), (y2[i_], d2i)):
                    fw.dma("pool", lambda e: e.indirect_dma_start(out=yb_[:], out_offset=None, in_=YB[:, :], in_offset=bass.IndirectOffsetOnAxis(ap=dxi[:, t__:t__ + 1], axis=0)), [dxi], [yb_])
            cload(0); cload(1)
            for t_ in range(32):
                i = t_ % 3
                if t_ + 2 < 32:
                    cload(t_ + 2)
                stt(x1r[i][:], y1[i][:], G1[:, t_:t_ + 1], x1r[i][:], ALU.mult, ALU.add, [y1[i], G1, x1r[i]], [x1r[i]])
                stt(x1r[i][:], y2[i][:], G2[:, t_:t_ + 1], x1r[i][:], ALU.mult, ALU.add, [y2[i], G2, x1r[i]], [x1r[i]])
                fw.dma("sp", lambda e: e.dma_start(out=out[t_ * 128:(t_ + 1) * 128, :], in_=x1r[i][:]), [x1r[i]], [])
            fw.barrier()
    return nc, outs


def host_inputs(inputs):
    f = lambda a: np.ascontiguousarray(np.asarray(a, dtype=np.float32))
    x = f(inputs["x"])
    pv = lambda v: f(np.asarray(v).reshape(-1, 128).T)
    com = {
        "w_in": f(inputs["w_in"][0]),
        "g1p": pv(inputs["norm1_g"][0]), "g2p": pv(inputs["norm2_g"][0]),
        "wa2": f(inputs["w_gla_a2"][0]), "ba": pv(inputs["b_gla_a"][0]),
        "gog": pv(inputs["gla_out_norm_g"][0]),
        "dqg": f(np.asarray(inputs["dil_q_norm_g"][0]).T), "dkg": f(np.asarray(inputs["dil_k_norm_g"][0]).T),
        "wpg": f(inputs["w_proj_gla"][0]), "wpa": f(inputs["w_proj_attn"][0]),
        "wbg": f(inputs["w_branch_gate"][0]), "bbp": pv(inputs["b_branch_gate"][0]),
        "wo": f(inputs["w_out"][0]),
        "wr": f(np.concatenate([np.asarray(inputs["w_router_group"][0]), np.asarray(inputs["w_router_expert"][0])], axis=1)),
        "brb": f(np.tile(np.concatenate([np.asarray(inputs["b_router_group"][0]), np.asarray(inputs["b_router_expert"][0])])[None, :], (128, 1))),
        "wg_r": f(np.asarray(inputs["w_gate"][0]).reshape(NEXP, 8, 128, DEXP).transpose(0, 2, 1, 3).reshape(NEXP * 128, 8 * DEXP)),
        "wu_r": f(np.asarray(inputs["w_up"][0]).reshape(NEXP, 8, 128, DEXP).transpose(0, 2, 1, 3).reshape(NEXP * 128, 8 * DEXP)),
        "wd_r": f(np.asarray(inputs["w_down"][0]).reshape(NEXP, 4, 128, D).transpose(0, 2, 1, 3).reshape(NEXP * 128, 4 * D)),
    }
    j = np.arange(128)[:, None]; i = np.arange(128)[None, :]
    com["c_ident"] = f(np.eye(128)); com["c_causal"] = f(j <= i); com["c_ustrict"] = f(j < i)
    com["c_iota"] = f(np.arange(128)[:, None])
    import ml_dtypes
    bO = np.zeros((128, 12, 256), np.float64)
    c = np.arange(256)[None, :]
    for g in range(3):
        for h in range(4):
            sl = SLOPES[g * 4 + h] * DILS[g]
            dist = np.where(c < 128, c - j, c - 128 + 128 - j)
            valid = np.where(c < 128, (c - j) >= 0, j >= (c - 128))
            bO[:, g * 4 + h, :] = np.where(valid, -sl * dist, -30000.0)
    bf = lambda a: a.astype(np.float32).astype(ml_dtypes.bfloat16).astype(np.float64)
    bOh = bf(bO); bOl = bf(bO - bOh)
    com["c_bOh"] = f(bOh.reshape(128, -1)); com["c_bOl"] = f(bOl.reshape(128, -1))
    bHh1 = bOh[:, :, 128:256]; bHl1 = bOl[:, :, 128:256]
    bHh0 = np.full_like(bHh1, bf(np.array(-30000.0))); bHl0 = np.zeros_like(bHl1)
    maps = []
    for core in range(8):
        b, hf = core // 2, core % 2
        m = dict(com)
        m["xo"] = f(x[b, hf * NTOK:(hf + 1) * NTOK])
        m["xp"] = f(x[b, 0:NTOK]) if hf == 1 else np.zeros((NTOK, D), np.float32)
        m["c_bHh"] = f((bHh1 if hf == 1 else bHh0).reshape(128, -1)); m["c_bHl"] = f((bHl1 if hf == 1 else bHl0).reshape(128, -1))
        maps.append(m)
    return maps


_NC = {}


def kernel(**inputs):
    if "nc" not in _NC:
        _NC["nc"] = build()[0]
    maps = host_inputs(inputs)
    res = run_bass_kernel_spmd(_NC["nc"], maps, core_ids=list(range(8)))
    o = np.zeros((4, 8192, D), np.float32)
    for core in range(8):
        b, hf = core // 2, core % 2
        o[b, hf * NTOK:(hf + 1) * NTOK] = res.results[core]["out"]
    return o
```

```python
import numpy as np
import concourse.bass as bass
import concourse.mybir as mybir
from concourse.bass_utils import run_bass_kernel_spmd
from contextlib import ExitStack

F32 = mybir.dt.float32; BF16 = mybir.dt.bfloat16; I32 = mybir.dt.int32
AF = mybir.ActivationFunctionType; ALU = mybir.AluOpType; AX = mybir.AxisListType

D = 1024; NTOK = 4096; MGS = 2048; IN_COLS = 6672; EPS = 1e-6
NEXP = 32; DEXP = 512; NBLK = 96; PROWS = NBLK * 128
SLOPES = [2.0 ** (-8.0 * (i + 1) / 12) for i in range(12)]
DILS = (1, 4, 16)


def ss(s, n, st):
    return slice(s, s + (n - 1) * st + 1, st)


class T:
    __slots__ = ("lw", "rd")

    def __init__(self):
        self.lw = {}; self.rd = {}


class Buf:
    def __init__(self, h):
        self.h = h; self.t = T()

    def __getitem__(self, k):
        return self.h[k]


class Eng:
    def __init__(self, fw, name, eng):
        self.name = name; self.eng = eng
        self.sem = fw.nc.alloc_semaphore("s_" + name); self.cnt = 0
        self.seen = {}; self.dsems = []; self.dk = 0; self.pend = False


class FW:
    NDMA = 16

    def __init__(self, nc):
        self.nc = nc; self.E = {}
        for n, e in (("pe", nc.tensor), ("act", nc.scalar), ("dve", nc.vector), ("pool", nc.gpsimd), ("sp", nc.sync)):
            self.E[n] = Eng(self, n, e)
        self.allsems = {}

    def _wait(self, e, tok):
        sem, val = tok
        if e.seen.get(sem.name, 0) >= val:
            return
        e.eng.wait_ge(sem, val); e.seen[sem.name] = val

    @staticmethod
    def _ts(bufs):
        return [b.t if isinstance(b, Buf) else b for b in bufs]

    def _deps(self, e, reads, writes):
        best = {}

        def add(tok):
            n = tok[0].name
            if n not in best or best[n][1] < tok[1]:
                best[n] = tok
        for t in reads:
            for tok in t.lw.values():
                add(tok)
        for t in writes:
            if t.rd:
                for tok in list(t.rd.values()) + list(t.lw.values()):
                    if tok[0] is not e.sem:
                        add(tok)
        for tok in best.values():
            self._wait(e, tok)

    def _mark(self, tok, reads, writes):
        n = tok[0].name
        for t in writes:
            if t.rd:
                t.lw = {}; t.rd = {}
            t.lw[n] = tok
        for t in reads:
            t.rd[n] = tok

    def op(self, en, fn, reads=(), writes=(), inc=True):
        e = self.E[en]
        reads = self._ts(reads); writes = self._ts(writes)
        self._deps(e, reads, writes)
        ins = fn(e.eng)
        if inc:
            e.cnt += 1
            ins.then_inc(e.sem, 1)
            tok = (e.sem, e.cnt); e.pend = False
        else:
            tok = (e.sem, e.cnt + 1); e.pend = True
        self._mark(tok, reads, writes)
        return ins

    def dma(self, qn, fn, reads=(), writes=()):
        e = self.E[qn]
        reads = self._ts(reads); writes = self._ts(writes)
        self._deps(e, reads, writes)
        k = e.dk; e.dk += 1
        slot = k % self.NDMA
        if slot >= len(e.dsems):
            e.dsems.append(self.nc.alloc_semaphore("d_%s_%d" % (qn, slot)))
        sem = e.dsems[slot]; val = 16 * (k // self.NDMA + 1)
        if val > 16:
            self._wait(e, (sem, val - 16))
        ins = fn(e.eng)
        ins.then_inc(sem, 16)
        tok = (sem, val)
        self.allsems[sem.name] = tok
        self._mark(tok, reads, writes)
        return tok

    def barrier(self):
        toks = []
        for e in self.E.values():
            assert not e.pend
            if e.cnt:
                toks.append((e.sem, e.cnt))
        toks += list(self.allsems.values())
        for e in self.E.values():
            for tok in toks:
                if tok[0] is not e.sem:
                    self._wait(e, tok)


def build(upto="all", dbg=False):
    nc = bass.Bass("TRN2", target_bir_lowering=False)
    fw = FW(nc)
    outs = {}

    def din(name, shape, dt=F32):
        return nc.dram_tensor(name, list(shape), dt, kind="ExternalInput").ap()

    def dscr(name, shape, dt):
        if dbg:
            outs[name] = (shape, dt)
            return nc.dram_tensor(name, list(shape), dt, kind="ExternalOutput").ap()
        return nc.dram_tensor(name, list(shape), dt).ap()

    xo = din("xo", [NTOK, D]); xp = din("xp", [NTOK, D])
    w_in = din("w_in", [D, IN_COLS])
    g1p = din("g1p", [128, 8]); g2p = din("g2p", [128, 8])
    wa2 = din("wa2", [16, 512]); ba = din("ba", [128, 4]); gog = din("gog", [128, 1])
    dqg = din("dqg", [128, 3]); dkg = din("dkg", [128, 3])
    wpg = din("wpg", [512, D]); wpa = din("wpa", [512, D]); wbg = din("wbg", [D, 2048]); bbp = din("bbp", [128, 16])
    wo = din("wo", [D, D]); wr = din("wr", [D, 36]); brb = din("brb", [128, 36])
    wg_r = din("wg_r", [NEXP * 128, 8 * DEXP]); wu_r = din("wu_r", [NEXP * 128, 8 * DEXP]); wd_r = din("wd_r", [NEXP * 128, 4 * D])
    c_ident = din("c_ident", [128, 128]); c_causal = din("c_causal", [128, 128]); c_ustrict = din("c_ustrict", [128, 128])
    c_iota = din("c_iota", [128, 1]); c_bOh = din("c_bOh", [128, 12 * 256]); c_bOl = din("c_bOl", [128, 12 * 256]); c_bHh = din("c_bHh", [128, 12 * 128]); c_bHl = din("c_bHl", [128, 12 * 128])
    out = nc.dram_tensor("out", [NTOK, D], F32, kind="ExternalOutput").ap()

    GQ = dscr("GQ", [4, 128, NTOK], BF16); GK = dscr("GK", [4, 128, 2 * NTOK], BF16); GR = dscr("GR", [4, 128, NTOK], BF16)
    LA = dscr("LA", [4, 128, 2 * NTOK], F32); GV = dscr("GV", [64, 128, 512], BF16)
    DQ = [dscr("DQ%d" % g, [4, 128, NTOK], BF16) for g in range(3)]
    DK = [dscr("DK%d" % g, [4, 128, 2 * NTOK], BF16) for g in range(3)]
    DV = [dscr("DV%d" % g, [4, 128, 48, 128], BF16) for g in range(3)]
    HT = dscr("HT", [8, 128, NTOK], BF16)
    OG = dscr("OG", [4, 128, NTOK], BF16); OA = dscr("OA", [4, 128, NTOK], BF16)
    X1 = dscr("X1", [NTOK, D], F32); XN = dscr("XN", [NTOK, D], BF16)
    XS = dscr("XS", [PROWS, D], BF16); YB = dscr("YB", [PROWS, D], F32)
    WGS = dscr("WGS", [NEXP * 128, 8 * DEXP], BF16); WUS = dscr("WUS", [NEXP * 128, 8 * DEXP], BF16); WDS = dscr("WDS", [NEXP * 128, 4 * D], BF16)

    cnt = [0]

    def nm(p):
        cnt[0] += 1
        return "%s_%d" % (p, cnt[0])

    def act(o, i, func, R, W, **kw):
        return fw.op("act", lambda e: e.activation(out=o, in_=i, func=func, **kw), R, W)

    def mm(o, lhsT, rhs, st, sp, R, W, inc=None):
        return fw.op("pe", lambda e: e.matmul(o, lhsT=lhsT, rhs=rhs, start=st, stop=sp), R, W, inc=(sp if inc is None else inc))

    def tr(o, i, ident, R, W, inc=True):
        return fw.op("pe", lambda e: e.transpose(out=o, in_=i, identity=ident), R, W, inc=inc)

    def ld(o, i, W, R=()):
        return fw.dma("sp", lambda e: e.dma_start(out=o, in_=i), R, W)

    def st(o, i, R, W=()):
        return fw.dma("pool", lambda e: e.dma_start(out=o, in_=i), R, W)

    def tt(en, o, a, b, op, R, W):
        return fw.op(en, lambda e: e.tensor_tensor(out=o, in0=a, in1=b, op=op), R, W)

    def tsc(en, o, a, s1, s2, op0, op1, R, W, **kw):
        if en == "pool" and op0 == ALU.mult and op1 == ALU.bypass:
            s2 = 0.0; op1 = ALU.add
        return fw.op(en, lambda e: e.tensor_scalar(out=o, in0=a, scalar1=s1, scalar2=s2, op0=op0, op1=op1, **kw), R, W)

    def stt(o, a, s, b, op0, op1, R, W):
        return fw.op("dve", lambda e: e.scalar_tensor_tensor(out=o, in0=a, scalar=s, in1=b, op0=op0, op1=op1), R, W)

    def cp(en, o, i, R, W):
        if en == "act":
            return fw.op(en, lambda e: e.activation(out=o, in_=i, func=AF.Copy), R, W)
        return fw.op(en, lambda e: e.tensor_copy(out=o, in_=i), R, W)

    with ExitStack() as top:
        def SB(stack, name, shape, dt):
            return Buf(stack.enter_context(nc.sbuf_tensor(nm(name), list(shape), dt)))

        def PS(stack, name, shape, dt):
            return Buf(stack.enter_context(nc.psum_tensor(nm(name), list(shape), dt)))

        identf = SB(top, "identf", [128, 128], F32); identb = SB(top, "identb", [128, 128], BF16)
        onesb = SB(top, "onesb", [128, 128], BF16)
        g1s = SB(top, "g1s", [128, 8], F32); g2s = SB(top, "g2s", [128, 8], F32)
        ld(identf[:], c_ident[:, :], [identf]); ld(g1s[:], g1p[:, :], [g1s]); ld(g2s[:], g2p[:, :], [g2s])
        cp("dve", identb[:], identf[:], [identf], [identb])
        fw.op("pool", lambda e: e.memset(onesb[:], 1.0), [], [onesb])
        zt = SB(top, "zt", [128, D], BF16)
        fw.op("pool", lambda e: e.memset(zt[:], 0.0), [], [zt])
        for h_ in range(4):
            st(DV[0][h_, :, 0:8, :], zt[:, 0:1024].rearrange("p (t e) -> p t e", e=128), [zt])
            st(DV[0][h_, :, 8:15, :], zt[:, 0:896].rearrange("p (t e) -> p t e", e=128), [zt])
            for r_ in range(4):
                st(DV[1][h_, :, r_ * 4:r_ * 4 + 3, :], zt[:, 0:384].rearrange("p (t e) -> p t e", e=128), [zt])
        xs_fill = [0]

        def xs_zero(n_):
            for _ in range(n_):
                if xs_fill[0] < NBLK:
                    b_ = xs_fill[0]; xs_fill[0] += 1
                    st(XS[b_ * 128:(b_ + 1) * 128, :], zt[:], [zt])

        with ExitStack() as ph:
            wib = SB(ph, "wib", [128, 8, IN_COLS], BF16)
            stg = [SB(ph, "wstg", [128, 1668], F32) for _ in range(4)]
            w_v = w_in.rearrange("(kc p) n -> p kc n", p=128)
            k = 0
            for kc in range(8):
                for hf in range(4):
                    s = stg[k % 4]
                    ld(s[:], w_v[:, kc, hf * 1668:(hf + 1) * 1668], [s])
                    if k % 4 == 3:
                        act(wib[:, kc, hf * 1668:(hf + 1) * 1668], s[:], AF.Copy, [s, g1s], [wib], scale=g1s[:, kc:kc + 1])
                    else:
                        en = "dve" if k % 2 == 0 else "pool"
                        tsc(en, wib[:, kc, hf * 1668:(hf + 1) * 1668], s[:], g1s[:, kc:kc + 1], None, ALU.mult, ALU.bypass, [s, g1s], [wib])
                    k += 1
            wa2s = SB(ph, "wa2s", [16, 512], F32); bas = SB(ph, "bas", [128, 4], F32); nbas = SB(ph, "nbas", [128, 4], F32)
            dqs = SB(ph, "dqs", [128, 3], F32); dks = SB(ph, "dks", [128, 3], F32); dqss = SB(ph, "dqss", [128, 3], F32)
            ld(wa2s[:], wa2[:, :], [wa2s]); ld(bas[:], ba[:, :], [bas]); ld(dqs[:], dqg[:, :], [dqs]); ld(dks[:], dkg[:, :], [dks])
            tsc("dve", nbas[:], bas[:], -1.0, None, ALU.mult, ALU.bypass, [bas], [nbas])
            tsc("dve", dqss[:], dqs[:], 128.0 ** -0.5, None, ALU.mult, ALU.bypass, [dqs], [dqss])
            hT = SB(ph, "hT", [128, 8, MGS], BF16)
            xt = [SB(ph, "xt", [128, D], F32) for _ in range(3)]
            xn = [SB(ph, "xn", [128, D], BF16) for _ in range(2)]
            junk = SB(ph, "junk", [128, D], BF16)
            ssq = [SB(ph, "ssq", [128, 1], F32) for _ in range(2)]
            lnv = [SB(ph, "lnv", [128, 1], F32) for _ in range(2)]
            rstd = [SB(ph, "rstd", [128, 1], F32) for _ in range(2)]
            ob = [SB(ph, "ob", [128, 512], BF16) for _ in range(4)]
            of = [SB(ph, "of", [128, 512], F32) for _ in range(2)]
            sq = [SB(ph, "sq", [128, 512], BF16) for _ in range(3)]
            t1 = [SB(ph, "t1", [128, 512], F32) for _ in range(2)]
            t2 = [SB(ph, "t2", [128, 512], F32) for _ in range(2)]
            gaf = SB(ph, "gaf", [16, 512], F32)
            ptr = PS(ph, "ptr", [128, 1024], BF16)
            pp = [PS(ph, "pp", [128, 512], F32) for _ in range(4)]
            pst = [PS(ph, "pst", [128, 512], F32) for _ in range(2)]
            pga = PS(ph, "pga", [128, 512], F32)
            rr = {"ob": 0, "of": 0, "pp": 0, "sq": 0, "pst": 0, "x": 0}

            pendq = []

            def nxt(key, lst):
                r = lst[rr[key] % len(lst)]; rr[key] += 1
                return r

            def proj_fm(col0, ncol, tok0):
                p = nxt("pp", pp)
                for kc in range(8):
                    mm(p[0:ncol, :], wib[:, kc, col0:col0 + ncol], hT[:, kc, tok0:tok0 + 512], kc == 0, kc == 7, [wib, hT], [p])
                return p

            def proj_tm(col0, tsl):
                p = nxt("pp", pp)
                for kc in range(8):
                    mm(p[:, :], hT[:, kc, tsl], wib[:, kc, col0:col0 + 512], kc == 0, kc == 7, [wib, hT], [p])
                return p

            def xload(gidx):
                q_ = gidx // 16; tix_ = gidx % 16
                src_ = xo if q_ >= 2 else xp
                base_ = (q_ - 2) * MGS if q_ >= 2 else q_ * MGS
                ld(xt[gidx % 3][:], src_[base_ + tix_ * 128: base_ + (tix_ + 1) * 128, :], [xt[gidx % 3]])

            def stageA(gidx):
                i_ = gidx % 2; x_ = xt[gidx % 3]
                act(junk[:], x_[:], AF.Square, [x_, junk], [junk, ssq[i_]], accum_out=ssq[i_][:])
                act(lnv[i_][:], ssq[i_][:], AF.Ln, [ssq[i_]], [lnv[i_]], scale=1.0 / D, bias=EPS)
                act(rstd[i_][:], lnv[i_][:], AF.Exp, [lnv[i_]], [rstd[i_]], scale=-0.5)
            xload(0); xload(1); stageA(0)
            for q in range(4):
                own = q >= 2
                ext0 = q * MGS
                for tix in range(16):
                    gidx = q * 16 + tix
                    i = gidx % 2
                    if gidx + 2 < 64:
                        xload(gidx + 2)
                    if gidx + 1 < 64:
                        stageA(gidx + 1)
                    x_ = xt[gidx % 3]
                    tsc("dve", xn[i][:], x_[:], rstd[i][:, 0:1], None, ALU.mult, ALU.bypass, [x_, rstd[i]], [xn[i]])
                    for kc in range(8):
                        tr(ptr[:, kc * 128:(kc + 1) * 128], xn[i][:, kc * 128:(kc + 1) * 128], identb[:], [xn[i], identb], [ptr], inc=(kc == 7))
                    cp("act" if tix % 2 else "dve", hT[:, :, tix * 128:(tix + 1) * 128], ptr[:, :].rearrange("p (k t) -> p k t", k=8), [ptr], [hT])
                if own:
                    o0 = (q - 2) * MGS
                    for kc in range(8):
                        st(HT[kc, :, o0:o0 + MGS], hT[:, kc, :], [hT])
                for sg in range(4):
                    tok0 = sg * 512; e0 = ext0 + tok0
                    for h in range(4):
                        p = proj_fm(512 + h * 128, 128, tok0)
                        o = nxt("ob", ob)
                        cp("act", o[:], p[:, :], [p], [o])
                        st(GK[h, :, e0:e0 + 512], o[:], [o])
                    for kc in range(8):
                        mm(pga[0:16, :], wib[:, kc, 2048:2064], hT[:, kc, tok0:tok0 + 512], kc == 0, kc == 7, [wib, hT], [pga])
                    cp("dve", gaf[:], pga[0:16, :], [pga], [gaf])
                    for h in range(4):
                        p = nxt("pp", pp)
                        mm(p[:, :], wa2s[:, h * 128:(h + 1) * 128], gaf[:], True, True, [wa2s, gaf], [p])
                        a = nxt("of", of)
                        act(a[:], p[:, :], AF.Exp, [p, nbas], [a], scale=-1.0, bias=nbas[:, h:h + 1])
                        act(a[:], a[:], AF.Ln, [a], [a], bias=1.0)
                        st(LA[h, :, e0:e0 + 512], a[:], [a])
                for tix in range(16):
                    p = proj_tm(1024, slice(tix * 128, (tix + 1) * 128))
                    o = nxt("ob", ob)
                    cp("act" if tix % 2 else "dve", o[:], p[:, :], [p], [o])
                    st(GV[q * 16 + tix, :, :], o[:], [o])
                if own:
                    o0 = (q - 2) * MGS
                    for sg in range(4):
                        tok0 = sg * 512
                        for h in range(4):
                            p = proj_fm(h * 128, 128, tok0)
                            o = nxt("ob", ob)
                            act(o[:], p[:, :], AF.Copy, [p], [o], scale=128.0 ** -0.5)
                            st(GQ[h, :, o0 + tok0:o0 + tok0 + 512], o[:], [o])
                    for sg in range(4):
                        tok0 = sg * 512
                        for h in range(4):
                            p = proj_fm(1536 + h * 128, 128, tok0)
                            o = nxt("ob", ob)
                            act(o[:], p[:, :], AF.Silu, [p], [o])
                            st(GR[h, :, o0 + tok0:o0 + tok0 + 512], o[:], [o])
                if q >= 1:
                    for g in range(3):
                        d = DILS[g]
                        cbase = 2064 + g * 1536
                        sgs = range(4) if (own or g == 2) else [3]
                        for which in ((0, 1) if own else (1,)):
                            for sg in sgs:
                                tok0 = sg * 512
                                for h in range(4):
                                    p = proj_fm(cbase + which * 512 + h * 128, 128, tok0)
                                    s_ = nxt("sq", sq)
                                    act(s_[:], p[:, :], AF.Square, [p], [s_])

                                    def post(p=p, s_=s_, which=which, g=g, h=h, tok0=tok0):
                                        ps_ = nxt("pst", pst)
                                        mm(ps_[:, :], onesb[:], s_[:], True, True, [onesb, s_], [ps_])
                                        a = t1[rr["pst"] % 2]; b = t2[rr["pst"] % 2]
                                        act(a[:], ps_[:, :], AF.Ln, [ps_], [a], scale=1.0 / 128, bias=EPS)
                                        act(b[:], a[:], AF.Exp, [a], [b], scale=-0.5)
                                        o = nxt("ob", ob)
                                        gsc = (dqss if which == 0 else dks)
                                        stt(o[:], p[:, :], gsc[:, g:g + 1], b[:], ALU.mult, ALU.mult, [p, gsc, b], [o])
                                        if which == 0:
                                            st(DQ[g][h, :, (q - 2) * MGS + tok0:(q - 2) * MGS + tok0 + 512], o[:], [o])
                                        else:
                                            st(DK[g][h, :, ext0 + tok0:ext0 + tok0 + 512], o[:], [o])
                                    if pendq:
                                        pendq.pop(0)()
                                    pendq.append(post)
                        while pendq:
                            pendq.pop(0)()
                        nbm = 16 // d
                        for r in range(d):
                            for ml in range(nbm):
                                if not own and ml != nbm - 1:
                                    continue
                                p = proj_tm(cbase + 1024, ss((ml * 128) * d + r, 128, d))
                                o = nxt("ob", ob)
                                cp("act" if ml % 2 else "dve", o[:], p[:, :], [p], [o])
                                st(DV[g][:, :, (q - 1) * 16 + r * nbm + ml, :].rearrange("h p e -> p h e"), o[:].rearrange("p (h e) -> p h e", h=4), [o])
            fw.barrier()
        if upto == "P":
            return nc, outs

        mid = top.enter_context(ExitStack())
        cst = [SB(mid, "cst", [128, 2048], F32) for _ in range(3)]
        ccb = [SB(mid, "ccb", [128, 2048], BF16) for _ in range(3)]

        def conv_gen():
            chunks = [(e_, src, dst, scaled, hf) for e_ in range(NEXP)
                      for (src, dst, scaled) in ((wg_r, WGS, True), (wu_r, WUS, True), (wd_r, WDS, False)) for hf in range(2)]

            def cld(k_):
                e_, src, dst, scaled, hf = chunks[k_]
                s_ = cst[k_ % 3]
                ld(s_[:], src[e_ * 128:(e_ + 1) * 128, hf * 2048:(hf + 1) * 2048], [s_])
            cld(0)
            for k_ in range(len(chunks)):
                e_, src, dst, scaled, hf = chunks[k_]
                s_ = cst[k_ % 3]; c_ = ccb[k_ % 3]
                if k_ + 1 < len(chunks):
                    cld(k_ + 1)
                for kk in range(4):
                    kc = hf * 4 + kk
                    sl_ = slice(kk * 512, (kk + 1) * 512)
                    tsc("pool", c_[:, sl_], s_[:, sl_], (g2s[:, kc:kc + 1] if scaled else 1.0), None, ALU.mult, ALU.bypass, [s_, g2s], [c_])
                st(dst[e_ * 128:(e_ + 1) * 128, hf * 2048:(hf + 1) * 2048], c_[:], [c_])
                yield
        conv_eng = ["act"]
        conv = conv_gen()

        def conv_step(n_=1):
            for _ in range(n_):
                next(conv, None)

        with ExitStack() as ph:
            gogs = SB(ph, "gogs", [128, 1], F32); causf = SB(ph, "causf", [128, 128], F32)
            caus4 = SB(ph, "caus4", [128, 4, 128], F32)
            rmask = SB(ph, "rmask", [128, 512], F32)
            ld(gogs[:], gog[:, :], [gogs]); ld(causf[:], c_causal[:, :], [causf])
            for h in range(4):
                cp("pool", caus4[:, h, :], causf[:], [causf], [caus4])
            fw.op("pool", lambda e: e.memset(rmask[:], 1.0), [], [rmask])
            fw.op("pool", lambda e: e.memset(rmask[:, ss(0, 4, 128)], 0.0), [rmask], [rmask])
            S = SB(ph, "S", [128, 4, 128], F32); Sb = SB(ph, "Sb", [128, 4, 128], BF16); Dt = SB(ph, "Dt", [128, 4, 128], F32)
            fw.op("pool", lambda e: e.memset(S[:], 0.0), [], [S])
            fw.op("pool", lambda e: e.memset(Sb[:], 0.0), [], [Sb])
            kT = [SB(ph, "kT", [128, 4, 512], BF16) for _ in range(3)]
            la = [SB(ph, "la", [128, 4, 512], F32) for _ in range(3)]
            vv = [SB(ph, "vv", [128, 4, 512], BF16) for _ in range(2)]
            qT = [SB(ph, "qT", [128, 4, 512], BF16) for _ in range(3)]
            grt = [SB(ph, "grt", [128, 4, 512], BF16) for _ in range(2)]
            bc = [SB(ph, "bc", [128, 4, 512], F32) for _ in range(2)]
            en_ = [SB(ph, "en", [128, 4, 512], F32)] * 2; eb_ = [SB(ph, "eb", [128, 4, 512], F32)] * 2
            dec = [SB(ph, "dec", [128, 4, 4], F32) for _ in range(2)]
            kin = [SB(ph, "kin", [128, 4, 512], BF16) for _ in range(2)]; qin = [SB(ph, "qin", [128, 4, 512], BF16) for _ in range(2)]
            kint = [SB(ph, "kint", [128, 512], BF16) for _ in range(4)]
            attm = [SB(ph, "attm", [128, 4, 128], BF16) for _ in range(4)]
            sqo = [SB(ph, "sqo", [128, 512], BF16) for _ in range(2)]
            lno = [SB(ph, "lno", [128, 512], F32) for _ in range(2)]
            rso = [SB(ph, "rso", [128, 512], F32) for _ in range(2)]
            onf = [SB(ph, "onf", [128, 512], F32) for _ in range(2)]
            ogb = [SB(ph, "ogb", [128, 4, 512], BF16) for _ in range(2)]
            pkt = [PS(ph, "pkt", [128, 512], BF16) for _ in range(2)]
            patt = [PS(ph, "patt", [128, 512], F32) for _ in range(2)]
            po = [PS(ph, "po", [128, 512], F32) for _ in range(2)]
            pS = PS(ph, "pS", [128, 512], F32)
            pn = PS(ph, "pn", [128, 512], F32)
            GKv = GK.rearrange("h p t -> p h t"); LAv = LA.rearrange("h p t -> p h t")
            GQv = GQ.rearrange("h p t -> p h t"); GRv = GR.rearrange("h p t -> p h t"); OGv = OG.rearrange("h p t -> p h t")
            ci = 0; pendg = []
            def gloadA(G_):
                i3 = G_ % 3; e0_ = G_ * 512
                ld(kT[i3][:], GKv[:, :, e0_:e0_ + 512], [kT[i3]])
                ld(la[i3][:], LAv[:, :, e0_:e0_ + 512], [la[i3]])
                if G_ >= 8:
                    o0_ = (G_ - 8) * 512
                    ld(qT[i3][:], GQv[:, :, o0_:o0_ + 512], [qT[i3]])

            def gloadB(G_):
                i_ = G_ % 2
                ld(vv[i_][:], GV[G_ * 4:(G_ + 1) * 4, :, :].rearrange("c p n -> p c n"), [vv[i_]])
                if G_ >= 8:
                    o0_ = (G_ - 8) * 512
                    ld(grt[i_][:], GRv[:, :, o0_:o0_ + 512], [grt[i_]])
            def preA(G_, h):
                i_ = G_ % 2; i3 = G_ % 3
                fw.op("dve", lambda e: e.tensor_tensor_scan(out=bc[i_][:, h, :], data0=rmask[:, 0:512], data1=la[i3][:, h, :], initial=0.0, op0=ALU.mult, op1=ALU.add), [rmask, la[i3]], [bc[i_]])
                act(en_[i_][:, h, :], bc[i_][:, h, :], AF.Exp, [bc[i_]], [en_[i_]], scale=1.0 / 16)
                act(dec[i_][:, h, :], bc[i_][:, h, :].rearrange("p (c t) -> p c t", c=4)[:, :, 127], AF.Exp, [bc[i_]], [dec[i_]], scale=-1.0 / 16)
                tt("dve", kin[i_][:, h, :], kT[i3][:, h, :], en_[i_][:, h, :], ALU.mult, [kT[i3], en_[i_]], [kin[i_]])
                if G_ >= 8:
                    act(eb_[i_][:, h, :], bc[i_][:, h, :], AF.Exp, [bc[i_]], [eb_[i_]], scale=-1.0 / 16)
                    tt("pool", qin[i_][:, h, :], qT[i3][:, h, :], eb_[i_][:, h, :], ALU.mult, [qT[i3], eb_[i_]], [qin[i_]])
            gloadA(0); gloadB(0); gloadA(1)
            for h in range(4):
                preA(0, h)
            for G in range(16):
                own = G >= 8
                i = G % 2
                e0 = G * 512
                if G + 2 < 16:
                    gloadA(G + 2)
                if G + 1 < 16:
                    gloadB(G + 1)
                if own:
                    o0 = (G - 8) * 512
                for c in range(4):
                    cs = slice(c * 128, (c + 1) * 128)
                    for h in range(4):
                        tr(pkt[c % 2][:, h * 128:(h + 1) * 128], kin[i][:, h, cs], identb[:], [kin[i], identb], [pkt[c % 2]], inc=(h == 3))
                    cp("act", kint[c][:], pkt[c % 2][:, :], [pkt[c % 2]], [kint[c]])
                    if own:
                        for h in range(4):
                            mm(patt[c % 2][:, h * 128:(h + 1) * 128], kin[i][:, h, cs], qin[i][:, h, cs], True, True, [kin[i], qin[i]], [patt[c % 2]], inc=(h == 3))
                        tt("dve", attm[c][:], patt[c % 2][:, :].rearrange("p (h t) -> p h t", h=4), caus4[:], ALU.mult, [patt[c % 2], caus4], [attm[c]])
                for c in range(4):
                    cs = slice(c * 128, (c + 1) * 128)
                    j = ci % 2; ci += 1
                    if own:
                        for h in range(4):
                            mm(po[j][:, h * 128:(h + 1) * 128], vv[i][:, c, h * 128:(h + 1) * 128], attm[c][:, h, :], True, False, [vv[i], attm[c]], [po[j]], inc=False)
                            mm(po[j][:, h * 128:(h + 1) * 128], Sb[:, h, :], qin[i][:, h, cs], False, True, [Sb, qin[i]], [po[j]], inc=(h == 3))
                    for h in range(4):
                        mm(pS[:, h * 128:(h + 1) * 128], kint[c][:, h * 128:(h + 1) * 128], vv[i][:, c, h * 128:(h + 1) * 128], True, True, [kint[c], vv[i]], [pS], inc=(h == 3))
                    tt("dve", Dt[:], pS[:, :].rearrange("p (h t) -> p h t", h=4), S[:], ALU.add, [pS, S], [Dt])
                    decb = dec[i][:, :, c:c + 1].broadcast_to([128, 4, 128])
                    for h in range(4):
                        act(Sb[:, h, :], Dt[:, h, :], AF.Copy, [Dt, dec[i], Sb], [Sb], scale=dec[i][:, h, c:c + 1])
                    if c % 2 == 0:
                        conv_step()
                    xs_zero(2)
                    tt("dve", S[:], Dt[:], decb, ALU.mult, [Dt, dec[i]], [S])
                    if G + 1 < 16:
                        preA(G + 1, c)
                    if own:
                        act(sqo[j][:], po[j][:, :], AF.Square, [po[j]], [sqo[j]])

                        def gpost(j=j, i=i, c=c, cs=cs, o0=o0):
                            mm(pn[:, :], onesb[:], sqo[j][:], True, True, [onesb, sqo[j]], [pn])
                            act(lno[j][:], pn[:, :], AF.Ln, [pn], [lno[j]], scale=1.0 / 128, bias=EPS)
                            act(rso[j][:], lno[j][:], AF.Exp, [lno[j]], [rso[j]], scale=-0.5)
                            stt(onf[j][:], po[j][:, :], gogs[:, 0:1], rso[j][:], ALU.mult, ALU.mult, [po[j], gogs, rso[j]], [onf[j]])
                            tt("pool", ogb[i][:, :, cs], onf[j][:].rearrange("p (h t) -> p h t", h=4), grt[i][:, :, cs], ALU.mult, [onf[j], grt[i]], [ogb[i]])
                            if c == 3:
                                st(OGv[:, :, o0:o0 + 512], ogb[i][:], [ogb[i]])
                        if pendg:
                            pendg.pop(0)()
                        pendg.append(gpost)
                while pendg:
                    pendg.pop(0)()
            while pendg:
                pendg.pop(0)()
            fw.barrier()
        if upto == "G":
            return nc, outs

        with ExitStack() as ph:
            bOf = SB(ph, "bOf", [128, 12 * 256], F32); bHf = SB(ph, "bHf", [128, 12 * 128], F32)
            bOh = SB(ph, "bOh", [128, 12, 256], BF16); bOl = SB(ph, "bOl", [128, 12, 256], BF16)
            bHh = SB(ph, "bHh", [128, 12, 128], BF16); bHl = SB(ph, "bHl", [128, 12, 128], BF16)
            for (srcO, srcH, dO, dH) in ((c_bOh, c_bHh, bOh, bHh), (c_bOl, c_bHl, bOl, bHl)):
                ld(bOf[:], srcO[:, :], [bOf]); ld(bHf[:], srcH[:, :], [bHf])
                cp("dve", dO[:].rearrange("p a b -> p (a b)"), bOf[:], [bOf], [dO])
                cp("dve", dH[:].rearrange("p a b -> p (a b)"), bHf[:], [bHf], [dH])
            acc = SB(ph, "acc", [128, 2, NTOK], F32)
            kTd = [SB(ph, "kTd", [128, 2048 + NTOK], BF16) for _ in range(2)]
            qTd = [SB(ph, "qTd", [128, NTOK], BF16) for _ in range(2)]
            va = [SB(ph, "va", [128, 48, 128], BF16) for _ in range(2)]
            pm = [SB(ph, "pm", [128, 256], BF16) for _ in range(4)]
            rden = SB(ph, "rden", [128, NTOK], F32)
            oab = SB(ph, "oab", [128, NTOK], BF16)
            psc = [PS(ph, "psc", [128, 256], F32) for _ in range(4)]
            pov = [PS(ph, "pov", [128, 256], F32) for _ in range(3)]
            gi = 0; ri = 0; ki = 0; oi = 0
            hg_list = [(h_, g_) for h_ in range(4) for g_ in range(3)]
            hgr_list = [(h_, g_, r_) for (h_, g_) in hg_list for r_ in range(DILS[g_])]

            def kqload(ix):
                h_, g_ = hg_list[ix]; hb_ = 128 * DILS[g_]
                ld(kTd[ix % 2][:, 0:hb_ + NTOK], DK[g_][h_, :, NTOK - hb_:2 * NTOK], [kTd[ix % 2]])
                ld(qTd[ix % 2][:], DQ[g_][h_, :, :], [qTd[ix % 2]])
                ld(va[ix % 2][:], DV[g_][h_, :, :, :], [va[ix % 2]])
            kqload(0)
            conv_eng[0] = "pool"
            for h in range(4):
                for g in range(3):
                    d = DILS[g]; hb = 128 * d; nb = 32 // d; nbm = 16 // d
                    kt = kTd[gi % 2]; qt = qTd[gi % 2]; v = va[gi % 2]; gi += 1
                    if gi < len(hg_list):
                        kqload(gi)
                    mi = g * 4 + h
                    for r in range(d):
                        pms = {}

                        def vix(m, r=r, nbm=nbm):
                            if m < 0:
                                return r * nbm + nbm - 1
                            return 16 * (1 + m // nbm) + r * nbm + (m % nbm)

                        def QK(m):
                            nonlocal ki
                            if m < 0:
                                ks = ss(r, 128, d); qs = ss(r, 128, d); N = 128
                                bh = bHh[:, mi, :]; bl = bHl[:, mi, :]; bb_ = (bHh, bHl)
                            else:
                                ks = ss(hb + (m * 128) * d + r, 128, d)
                                N = 256 if m < nb - 1 else 128
                                qs = ss((m * 128) * d + r, N, d)
                                bh = bOh[:, mi, 0:N]; bl = bOl[:, mi, 0:N]; bb_ = (bOh, bOl)
                            p = psc[ki % 4]; pmb = pm[ki % 4]
                            if ki % 7 == 0:
                                conv_step()
                            ki += 1
                            mm(p[:, 0:N], kt[:, ks], qt[:, qs], True, False, [kt, qt], [p], inc=False)
                            mm(p[:, 0:N], identb[:], bh, False, False, [identb, bb_[0]], [p], inc=False)
                            mm(p[:, 0:N], identb[:], bl, False, True, [identb, bb_[1]], [p], inc=True)
                            act(pmb[:, 0:N], p[:, 0:N], AF.Exp, [p], [pmb])
                            pms[m] = pmb

                        def PV(m):
                            nonlocal oi
                            po_ = pov[oi % 3]; oi += 1
                            pprev = pms[m - 1]; pmb = pms[m]
                            rp = pprev[:, 0:128] if m == 0 else pprev[:, 128:256]
                            mm(po_[:, 0:128], v[:, vix(m - 1), :], rp, True, False, [v, pprev], [po_], inc=False)
                            mm(po_[:, 0:128], v[:, vix(m), :], pmb[:, 0:128], False, True, [v, pmb], [po_], inc=False)
                            mm(po_[:, 128:256], onesb[:], rp, True, False, [onesb, pprev], [po_], inc=False)
                            mm(po_[:, 128:256], onesb[:], pmb[:, 0:128], False, True, [onesb, pmb], [po_], inc=True)
                            av = acc[:, :, ss((m * 128) * d + r, 128, d)]
                            if g == 0:
                                cp("dve", av, po_[:, :].rearrange("p (a t) -> p a t", a=2), [po_], [acc])
                            else:
                                tt("dve", av, po_[:, :].rearrange("p (a t) -> p a t", a=2), av, ALU.add, [po_, acc], [acc])
                        blocks = list(range(-1, nb))
                        QK(blocks[0])
                        for bi, m in enumerate(blocks):
                            if bi + 1 < len(blocks):
                                QK(blocks[bi + 1])
                            if m >= 0:
                                PV(m)
                act(rden[:], acc[:, 1, :], AF.Ln, [acc], [rden])
                act(rden[:], rden[:], AF.Exp, [rden], [rden], scale=-1.0)
                tt("dve", oab[:], acc[:, 0, :], rden[:], ALU.mult, [acc, rden], [oab])
                st(OA[h, :, :], oab[:], [oab])
            conv_eng[0] = "act"
            fw.barrier()
        if upto == "D":
            return nc, outs

        OH1 = SB(top, "OH1", [128, 32, 32], F32); OH2 = SB(top, "OH2", [128, 32, 32], F32)
        G1 = SB(top, "G1", [128, 32], F32); G2 = SB(top, "G2", [128, 32], F32)
        LG = SB(top, "LG", [128, 32, 36], F32)
        with ExitStack() as ph:
            wpgb = SB(ph, "wpgb", [128, 4, D], BF16); wpab = SB(ph, "wpab", [128, 4, D], BF16)
            wbb = SB(ph, "wbb", [128, 8, 2048], BF16); wob = SB(ph, "wob", [128, 8, D], BF16)
            wrs = SB(ph, "wrs", [128, 8, 36], F32); brs = SB(ph, "brs", [128, 36], F32); bbs = SB(ph, "bbs", [128, 16], F32)
            ld(brs[:], brb[:, :], [brs]); ld(bbs[:], bbp[:, :], [bbs])
            with ExitStack() as wsc:
                stg = [SB(wsc, "mstg", [128, 2048], F32) for _ in range(2)]
                k = 0

                def wload(dst, src, rows_k, ncols, scale):
                    nonlocal k
                    v_ = src.rearrange("(kc p) n -> p kc n", p=128)
                    for kc in range(rows_k):
                        s = stg[k % 2]; k += 1
                        ld(s[:, 0:ncols], v_[:, kc, :], [s])
                        if scale is None:
                            cp("dve", dst[:, kc, :], s[:, 0:ncols], [s], [dst])
                        else:
                            tsc("dve", dst[:, kc, :], s[:, 0:ncols], scale[:, kc:kc + 1], None, ALU.mult, ALU.bypass, [s, scale], [dst])
                wload(wpgb, wpg, 4, D, None); wload(wpab, wpa, 4, D, None)
                wload(wbb, wbg, 8, 2048, g1s); wload(wob, wo, 8, D, None)
                wload(wrs, wr, 8, 36, g2s)
            fw.barrier()
            og = [SB(ph, "og", [128, 4, 512], BF16) for _ in range(2)]
            oa = [SB(ph, "oa", [128, 4, 512], BF16) for _ in range(2)]
            hTm = [SB(ph, "hTm", [128, 8, 512], BF16) for _ in range(2)]
            xr = [SB(ph, "xr", [128, D], F32) for _ in range(2)]
            sgt = [SB(ph, "sgt", [128, 512], F32) for _ in range(2)]
            sat = [SB(ph, "sat", [128, 512], F32) for _ in range(2)]
            m1t = [SB(ph, "m1t", [128, 512], F32)] * 2
            m2t = [SB(ph, "m2t", [128, 512], F32)] * 2
            zT = SB(ph, "zT", [128, 8, 512], BF16)
            x1t = [SB(ph, "x1t", [128, D], F32) for _ in range(2)]
            xn2 = [SB(ph, "xn2", [128, D], F32) for _ in range(2)]
            xn2b = [SB(ph, "xn2b", [128, D], BF16) for _ in range(2)]
            junk = SB(ph, "junkm", [128, D], BF16)
            xT2 = [SB(ph, "xT2", [128, 8, 128], F32) for _ in range(2)]
            sm = {k_: [SB(ph, k_, [128, w_], F32) for _ in range(2)] for k_, w_ in
                  (("ss2", 1), ("ln2", 1), ("rs2", 1))}
            pM = [PS(ph, "pM", [128, 512], F32) for _ in range(8)]
            OGv = OG.rearrange("h p t -> p h t"); OAv = OA.rearrange("h p t -> p h t"); HTv = HT.rearrange("k p t -> p k t")
            ti = 0; rq = []; rq2 = []
            def mload(Gp_):
                i_ = Gp_ % 2; o0_ = Gp_ * 512
                ld(og[i_][:], OGv[:, :, o0_:o0_ + 512], [og[i_]]); ld(oa[i_][:], OAv[:, :, o0_:o0_ + 512], [oa[i_]])
                ld(hTm[i_][:], HTv[:, :, o0_:o0_ + 512], [hTm[i_]])

            def xrload(t__):
                ld(xr[t__ % 2][:], xo[t__ * 128:(t__ + 1) * 128, :], [xr[t__ % 2]])
            mload(0); xrload(0)
            for Gp in range(8):
                i = Gp % 2; o0 = Gp * 512
                if Gp + 1 < 8:
                    mload(Gp + 1)
                for ct in range(8):
                    cs = slice(ct * 128, (ct + 1) * 128)
                    j = ct % 2
                    pA = [pM[4 * j]]; pB = [pM[4 * j + 1]]; pC = [pM[4 * j + 2]]; pD = [pM[4 * j + 3]]
                    conv_step()
                    for kc in range(4):
                        mm(pA[0][:, :], wpgb[:, kc, cs], og[i][:, kc, :], kc == 0, kc == 3, [wpgb, og[i]], [pA[0]])
                    for kc in range(4):
                        mm(pB[0][:, :], wpab[:, kc, cs], oa[i][:, kc, :], kc == 0, kc == 3, [wpab, oa[i]], [pB[0]])
                    for kc in range(8):
                        mm(pC[0][:, :], wbb[:, kc, cs], hTm[i][:, kc, :], kc == 0, kc == 7, [wbb, hTm[i]], [pC[0]])
                    for kc in range(8):
                        mm(pD[0][:, :], wbb[:, kc, D + ct * 128:D + (ct + 1) * 128], hTm[i][:, kc, :], kc == 0, kc == 7, [wbb, hTm[i]], [pD[0]])
                    act(sgt[j][:], pC[0][:, :], AF.Sigmoid, [pC[0], bbs], [sgt[j]], bias=bbs[:, ct:ct + 1])
                    act(sat[j][:], pD[0][:, :], AF.Sigmoid, [pD[0], bbs], [sat[j]], bias=bbs[:, 8 + ct:9 + ct])
                    tt("dve", m1t[j][:], pA[0][:, :], sgt[j][:], ALU.mult, [pA[0], sgt[j]], [m1t[j]])
                    tt("dve", m2t[j][:], pB[0][:, :], sat[j][:], ALU.mult, [pB[0], sat[j]], [m2t[j]])
                    tt("dve", zT[:, ct, :], m1t[j][:], m2t[j][:], ALU.add, [m1t[j], m2t[j]], [zT])
                for c in range(4):
                    t_ = Gp * 4 + c
                    j = ti % 2; ti += 1
                    pX = [pM[4 * j], pM[4 * j + 1]]; pT = pM[4 * j + 2]; pL = pM[4 * j + 3]
                    if t_ + 1 < 32:
                        xrload(t_ + 1)
                    conv_step()
                    for hf in range(2):
                        for kc in range(8):
                            mm(pX[hf][:, :], zT[:, kc, c * 128:(c + 1) * 128], wob[:, kc, hf * 512:(hf + 1) * 512], kc == 0, kc == 7, [zT, wob], [pX[hf]])
                        tt("dve", x1t[j][:, hf * 512:(hf + 1) * 512], pX[hf][:, :], xr[j][:, hf * 512:(hf + 1) * 512], ALU.add, [pX[hf], xr[j]], [x1t[j]])
                    st(X1[t_ * 128:(t_ + 1) * 128, :], x1t[j][:], [x1t[j]])
                    S_ = {k_: v_[j] for k_, v_ in sm.items()}
                    act(junk[:], x1t[j][:], AF.Square, [x1t[j], junk], [junk, S_["ss2"]], accum_out=S_["ss2"][:])
                    act(S_["ln2"][:], S_["ss2"][:], AF.Ln, [S_["ss2"]], [S_["ln2"]], scale=1.0 / D, bias=EPS)
                    act(S_["rs2"][:], S_["ln2"][:], AF.Exp, [S_["ln2"]], [S_["rs2"]], scale=-0.5)
                    tsc("dve", xn2[j][:], x1t[j][:], S_["rs2"][:, 0:1], None, ALU.mult, ALU.bypass, [x1t[j], S_["rs2"]], [xn2[j]])
                    tsc("pool", xn2b[j][:], xn2[j][:], 1.0, None, ALU.mult, ALU.bypass, [xn2[j]], [xn2b[j]])
                    st(XN[t_ * 128:(t_ + 1) * 128, :], xn2b[j][:], [xn2b[j]])
                    def router1(j=j, t_=t_, S_=S_, pT=pT, pL=pL):
                        for half, pb in ((0, pT), (1, pL)):
                            for kk in range(4):
                                kc = half * 4 + kk
                                tr(pb[:, kk * 128:(kk + 1) * 128], xn2[j][:, kc * 128:(kc + 1) * 128], identf[:], [xn2[j], identf], [pb], inc=(kk == 3))
                        for half, pb in ((0, pT), (1, pL)):
                            cp("act", xT2[j][:, half * 4:(half + 1) * 4, :], pb[:, :].rearrange("p (k t) -> p k t", k=4), [pb], [xT2[j]])
                        rq2.append(lambda: router2(j, t_, S_, pT))

                    def router2(j, t_, S_, pL):
                        for kc in range(8):
                            mm(pL[:, 0:36], xT2[j][:, kc, :], wrs[:, kc, :], kc == 0, kc == 7, [xT2[j], wrs], [pL])
                        tt("dve", LG[:, t_, :], pL[:, 0:36], brs[:], ALU.add, [pL, brs], [LG])

                    r2_ = rq2.pop(0) if rq2 else None
                    if rq:
                        rq.pop(0)()
                    if r2_ is not None:
                        r2_()
                    rq.append(router1)
            while rq or rq2:
                if rq2:
                    rq2.pop(0)()
                if rq:
                    rq.pop(0)()
            fw.barrier()
        if upto == "M":
            return nc, outs

        with ExitStack() as ph:
            conv_step(1000)
            fw.barrier()
            rsc = ExitStack()

            def RB(name, shape):
                return SB(rsc, name, shape, F32)
            gmax = RB("gmax", [128, 32]); ohg = RB("ohg", [128, 32, 4]); dlt = RB("dlt", [128, 32, 4]); exg = RB("exg", [128, 32, 4])
            sumex = RB("sumex", [128, 32]); gp = RB("gp", [128, 32]); pen = RB("pen", [128, 32, 4])
            msk = RB("msk", [128, 32, 32]); msk2 = RB("msk2", [128, 32, 32]); m1 = RB("m1", [128, 32]); m2 = RB("m2", [128, 32])
            dm = RB("dm", [128, 32]); e2 = RB("e2", [128, 32]); dn = RB("dn", [128, 32]); w1 = RB("w1", [128, 32]); w2 = RB("w2", [128, 32])
            glog = LG[:, :, 0:4]; elog = LG[:, :, 4:36]
            fw.op("dve", lambda e: e.reduce_max(out=gmax[:], in_=glog, axis=AX.X), [LG], [gmax])
            gmax_b = gmax[:].unsqueeze(2).broadcast_to([128, 32, 4])
            tt("dve", ohg[:], glog, gmax_b, ALU.is_ge, [LG, gmax], [ohg])
            tt("dve", dlt[:], glog, gmax_b, ALU.subtract, [LG, gmax], [dlt])
            act(exg[:], dlt[:], AF.Exp, [dlt], [exg])
            fw.op("dve", lambda e: e.reduce_sum(out=sumex[:], in_=exg[:], axis=AX.X), [exg], [sumex])
            fw.op("dve", lambda e: e.reciprocal(out=gp[:], in_=sumex[:]), [sumex], [gp])
            tsc("dve", pen[:], ohg[:], -1.0, 1e30, ALU.add, ALU.mult, [ohg], [pen])
            tt("dve", msk[:].rearrange("p t (g j) -> p t g j", g=4), elog.rearrange("p t (g j) -> p t g j", g=4), pen[:].unsqueeze(3).broadcast_to([128, 32, 4, 8]), ALU.add, [LG, pen], [msk])
            fw.op("dve", lambda e: e.reduce_max(out=m1[:], in_=msk[:], axis=AX.X), [msk], [m1])
            tt("dve", OH1[:], msk[:], m1[:].unsqueeze(2).broadcast_to([128, 32, 32]), ALU.is_ge, [msk, m1], [OH1])
            stt(msk2[:].rearrange("p t e -> p (t e)"), OH1[:].rearrange("p t e -> p (t e)"), -1e30, msk[:].rearrange("p t e -> p (t e)"), ALU.mult, ALU.add, [OH1, msk], [msk2])
            fw.op("dve", lambda e: e.reduce_max(out=m2[:], in_=msk2[:], axis=AX.X), [msk2], [m2])
            tt("dve", OH2[:], msk2[:], m2[:].unsqueeze(2).broadcast_to([128, 32, 32]), ALU.is_ge, [msk2, m2], [OH2])
            tt("dve", dm[:], m2[:], m1[:], ALU.subtract, [m1, m2], [dm])
            act(e2[:], dm[:], AF.Exp, [dm], [e2])
            tsc("dve", dn[:], e2[:], 1.0, None, ALU.add, ALU.bypass, [e2], [dn])
            fw.op("dve", lambda e: e.reciprocal(out=w1[:], in_=dn[:]), [dn], [w1])
            tt("dve", w2[:], w1[:], e2[:], ALU.mult, [w1, e2], [w2])
            tt("dve", G1[:], w1[:], gp[:], ALU.mult, [w1, gp], [G1])
            tt("dve", G2[:], w2[:], gp[:], ALU.mult, [w2, gp], [G2])
            fw.barrier()
            rsc.close()
            ustf = SB(ph, "ustf", [128, 128], F32); ustb = SB(ph, "ustb", [128, 128], BF16); iotas = SB(ph, "iotas", [128, 1], F32)
            ld(ustf[:], c_ustrict[:, :], [ustf]); ld(iotas[:], c_iota[:, :], [iotas])
            cp("dve", ustb[:], ustf[:], [ustf], [ustb])
            oh12 = SB(ph, "oh12", [128, 32, 32], BF16)
            tt("dve", oh12[:], OH1[:], OH2[:], ALU.add, [OH1, OH2], [oh12])
            bk = ExitStack()
            pex = [PS(bk, "pex", [128, 512], F32) for _ in range(2)]
            pcn = PS(bk, "pcn", [128, 512], F32)
            for t_ in range(32):
                pt = pex[t_ // 16]; sl = slice((t_ % 16) * 32, (t_ % 16 + 1) * 32)
                mm(pt[:, sl], ustb[:], oh12[:, t_, :], True, t_ == 0, [ustb, oh12], [pt], inc=(t_ == 0))
                for t2_ in range(t_):
                    mm(pt[:, sl], onesb[:], oh12[:, t2_, :], False, t2_ == t_ - 1, [onesb, oh12], [pt], inc=(t2_ == t_ - 1))
            for t_ in range(32):
                mm(pcn[:, 0:32], onesb[:], oh12[:, t_, :], t_ == 0, t_ == 31, [onesb, oh12], [pcn])
            cntf = SB(ph, "cntf", [128, 32], F32); cnti = SB(ph, "cnti", [128, 32], I32); pcf = SB(ph, "pcf", [128, 32], F32)
            pend = SB(ph, "pend", [128, 32], F32); pstart = SB(ph, "pstart", [128, 32], F32); ones32 = SB(ph, "ones32", [128, 32], F32)
            tsc("dve", cntf[:], pcn[:, 0:32], 127.0, None, ALU.add, ALU.bypass, [pcn], [cntf])
            cp("dve", cnti[:], cntf[:], [cntf], [cnti])
            tsc("dve", cnti[:], cnti[:], 7, 7, ALU.arith_shift_right, ALU.logical_shift_left, [cnti], [cnti])
            cp("dve", pcf[:], cnti[:], [cnti], [pcf])
            fw.op("pool", lambda e: e.memset(ones32[:], 1.0), [], [ones32])
            fw.op("dve", lambda e: e.tensor_tensor_scan(out=pend[:], data0=ones32[:], data1=pcf[:], initial=0.0, op0=ALU.mult, op1=ALU.add), [ones32, pcf], [pend])
            tt("dve", pstart[:], pend[:], pcf[:], ALU.subtract, [pend, pcf], [pstart])
            base = SB(ph, "base", [128, 32, 32], F32); prod = SB(ph, "prod", [128, 32, 32], F32)
            d1f = SB(ph, "d1f", [128, 32], F32); d2f = SB(ph, "d2f", [128, 32], F32)
            d1i = SB(ph, "d1i", [128, 32], I32); d2i = SB(ph, "d2i", [128, 32], I32)
            for t_ in range(32):
                pt = pex[t_ // 16]; sl = slice((t_ % 16) * 32, (t_ % 16 + 1) * 32)
                tt("dve", base[:, t_, :], pt[:, sl], pstart[:], ALU.add, [pt, pstart], [base])
            for (ohx, dxf, dxi) in ((OH1, d1f, d1i), (OH2, d2f, d2i)):
                tt("dve", prod[:], ohx[:], base[:], ALU.mult, [ohx, base], [prod])
                fw.op("dve", lambda e: e.reduce_sum(out=dxf[:], in_=prod[:], axis=AX.X), [prod], [dxf])
                cp("dve", dxi[:], dxf[:], [dxf], [dxi])
            bef = SB(ph, "bef", [128, NBLK], F32); bjunk = SB(ph, "bjunk", [128, 32], F32); idxw = SB(ph, "idxw", [128, NBLK], I32)
            for n in range(NBLK):
                tsc("dve", bjunk[:], pend[:], float(n * 128) + 0.5, None, ALU.is_le, ALU.add, [pend, bjunk], [bjunk, bef], accum_out=bef[:, n:n + 1])
            sf = SB(ph, "sf", [128, NBLK], F32)
            tsc("dve", bef[:], bef[:], 31.0, None, ALU.min, ALU.bypass, [bef], [bef])
            fw.op("pool", lambda e: e.memset(sf[:], 0.0), [], [sf])
            tt("dve", sf[:, 1:NBLK], bef[:, 1:NBLK], bef[:, 0:NBLK - 1], ALU.is_equal, [bef], [sf])
            for bnd in (NBLK // 3, 2 * NBLK // 3):
                fw.op("dve", lambda e: e.memset(sf[:, bnd:bnd + 1], 0.0), [sf], [sf])
            tsc("dve", bef[:], bef[:], 128.0, iotas[:, 0:1], ALU.mult, ALU.add, [bef, iotas], [bef])
            stt(bef[:], sf[:], 1.0e6, bef[:], ALU.mult, ALU.add, [sf, bef], [bef])
            cp("dve", idxw[:], bef[:], [bef], [idxw])
            fw.barrier()
            bk.close()
            xs_t = T()
            xg = [SB(ph, "xg", [128, D], BF16) for _ in range(2)]
            for t_ in range(32):
                b_ = xg[t_ % 2]
                ld(b_[:], XN[t_ * 128:(t_ + 1) * 128, :], [b_])
                for dxi in (d1i, d2i):
                    fw.dma("pool", lambda e: e.indirect_dma_start(out=XS[:, :], out_offset=bass.IndirectOffsetOnAxis(ap=dxi[:, t_:t_ + 1], axis=0), in_=b_[:], in_offset=None), [b_, dxi], [xs_t])
            fw.barrier()
            NL = 3; LB = NBLK // NL
            xin = [SB(ph, "xin", [128, D], BF16) for _ in range(4)]
            xinT = [SB(ph, "xinT", [128, 8, 128], BF16) for _ in range(2)]
            wgs = [SB(ph, "wgs", [128, 8, DEXP], BF16) for _ in range(NL)]
            wus = [SB(ph, "wus", [128, 8, DEXP], BF16) for _ in range(NL)]
            wds = [SB(ph, "wds", [128, 4, D], BF16) for _ in range(NL)]
            sgf = [SB(ph, "sgf", [128, DEXP], F32) for _ in range(2)]
            ab = [SB(ph, "ab", [128, DEXP], BF16) for _ in range(2)]
            aT = [SB(ph, "aT", [128, 4, 128], BF16) for _ in range(2)]
            ybs = [SB(ph, "ybs", [128, D], F32) for _ in range(2)]
            pxt = PS(ph, "pxt", [128, 1024], BF16)
            pg = PS(ph, "pg", [128, 512], F32); pu = PS(ph, "pu", [128, 512], F32)
            pat = PS(ph, "pat", [128, 512], BF16)
            py = [PS(ph, "py", [128, 512], F32) for _ in range(2)]
            bcreg = nc.gpsimd.to_reg(NEXP * 128 - 1)

            def blk(s_):
                return (s_ // NL) + LB * (s_ % NL)

            def SX(s_):
                n = blk(s_)
                ld(xin[s_ % 4][:], XS[n * 128:(n + 1) * 128, :], [xin[s_ % 4]])

            def SW(s_):
                n = blk(s_); i = s_ % NL
                for (wsb, wsrc) in ((wgs[i], WGS), (wus[i], WUS), (wds[i], WDS)):
                    fw.dma("pool", lambda e: e.indirect_dma_start(out=wsb[:].rearrange("p a b -> p (a b)"), out_offset=None, in_=wsrc[:, :], in_offset=bass.IndirectOffsetOnAxis(ap=idxw[:, n:n + 1], axis=0), bounds_check=bcreg, oob_is_err=False), [idxw], [wsb])

            def S1(s_):
                x_ = xin[s_ % 4]
                for kc in range(8):
                    tr(pxt[:, kc * 128:(kc + 1) * 128], x_[:, kc * 128:(kc + 1) * 128], identb[:], [x_, identb], [pxt], inc=(kc == 7))
                cp("dve", xinT[s_ % 2][:], pxt[:, :].rearrange("p (k t) -> p k t", k=8), [pxt], [xinT[s_ % 2]])

            def S2(s_):
                i = s_ % NL; j = s_ % 2
                for kc in range(8):
                    mm(pg[:, :], xinT[j][:, kc, :], wgs[i][:, kc, :], kc == 0, kc == 7, [xinT[j], wgs[i]], [pg])
                for kc in range(8):
                    mm(pu[:, :], xinT[j][:, kc, :], wus[i][:, kc, :], kc == 0, kc == 7, [xinT[j], wus[i]], [pu])
                act(sgf[j][:], pg[:, :], AF.Silu, [pg], [sgf[j]])
                tt("dve", ab[j][:], sgf[j][:], pu[:, :], ALU.mult, [sgf[j], pu], [ab[j]])

            def S3(s_):
                j = s_ % 2
                for fc in range(4):
                    tr(pat[:, fc * 128:(fc + 1) * 128], ab[j][:, fc * 128:(fc + 1) * 128], identb[:], [ab[j], identb], [pat], inc=(fc == 3))
                cp("dve", aT[j][:], pat[:, :].rearrange("p (k t) -> p k t", k=4), [pat], [aT[j]])

            def S4(s_):
                i = s_ % NL; j = s_ % 2; n = blk(s_)
                for hf in range(2):
                    for fc in range(4):
                        mm(py[hf][:, :], aT[j][:, fc, :], wds[i][:, fc, hf * 512:(hf + 1) * 512], fc == 0, fc == 3, [aT[j], wds[i]], [py[hf]])
                    cp("act", ybs[j][:, hf * 512:(hf + 1) * 512], py[hf][:, :], [py[hf]], [ybs[j]])
                st(YB[n * 128:(n + 1) * 128, :], ybs[j][:], [ybs[j]])
            for s_ in range(3):
                SX(s_); SW(s_)
            S1(0); S1(1); S2(0)
            for k_ in range(NBLK):
                S3(k_)
                if k_ + 1 < NBLK:
                    S2(k_ + 1)
                S4(k_)
                if k_ + 2 < NBLK:
                    S1(k_ + 2)
                if k_ + 3 < NBLK:
                    SW(k_ + 3); SX(k_ + 3)
            fw.barrier()
            y1 = [SB(ph, "y1", [128, D], F32) for _ in range(3)]
            y2 = [SB(ph, "y2", [128, D], F32) for _ in range(3)]
            x1r = [SB(ph, "x1r", [128, D], F32) for _ in range(3)]
            def cload(t__):
                i_ = t__ % 3
                ld(x1r[i_][:], X1[t__ * 128:(t__ + 1) * 128, :], [x1r[i_]])
                for (yb_, dxi) in ((y1[i_], d1i), (y2[i_], d2i)):
                    fw.dma("pool", lambda e: e.indirect_dma_start(out=yb_[:], out_offset=None, in_=YB[:, :], in_offset=bass.IndirectOffsetOnAxis(ap=dxi[:, t__:t__ + 1], axis=0)), [dxi], [yb_])
            cload(0); cload(1)
            for t_ in range(32):
                i = t_ % 3
                if t_ + 2 < 32:
                    cload(t_ + 2)
                stt(x1r[i][:], y1[i][:], G1[:, t_:t_ + 1], x1r[i][:], ALU.mult, ALU.add, [y1[i], G1, x1r[i]], [x1r[i]])
                stt(x1r[i][:], y2[i][:], G2[:, t_:t_ + 1], x1r[i][:], ALU.mult, ALU.add, [y2[i], G2, x1r[i]], [x1r[i]])
                fw.dma("sp", lambda e: e.dma_start(out=out[t_ * 128:(t_ + 1) * 128, :], in_=x1r[i][:]), [x1r[i]], [])
            fw.barrier()
    return nc, outs


def host_inputs(inputs):
    f = lambda a: np.ascontiguousarray(np.asarray(a, dtype=np.float32))
    x = f(inputs["x"])
    pv = lambda v: f(np.asarray(v).reshape(-1, 128).T)
    com = {
        "w_in": f(inputs["w_in"][0]),
        "g1p": pv(inputs["norm1_g"][0]), "g2p": pv(inputs["norm2_g"][0]),
        "wa2": f(inputs["w_gla_a2"][0]), "ba": pv(inputs["b_gla_a"][0]),
        "gog": pv(inputs["gla_out_norm_g"][0]),
        "dqg": f(np.asarray(inputs["dil_q_norm_g"][0]).T), "dkg": f(np.asarray(inputs["dil_k_norm_g"][0]).T),
        "wpg": f(inputs["w_proj_gla"][0]), "wpa": f(inputs["w_proj_attn"][0]),
        "wbg": f(inputs["w_branch_gate"][0]), "bbp": pv(inputs["b_branch_gate"][0]),
        "wo": f(inputs["w_out"][0]),
        "wr": f(np.concatenate([np.asarray(inputs["w_router_group"][0]), np.asarray(inputs["w_router_expert"][0])], axis=1)),
        "brb": f(np.tile(np.concatenate([np.asarray(inputs["b_router_group"][0]), np.asarray(inputs["b_router_expert"][0])])[None, :], (128, 1))),
        "wg_r": f(np.asarray(inputs["w_gate"][0]).reshape(NEXP, 8, 128, DEXP).transpose(0, 2, 1, 3).reshape(NEXP * 128, 8 * DEXP)),
        "wu_r": f(np.asarray(inputs["w_up"][0]).reshape(NEXP, 8, 128, DEXP).transpose(0, 2, 1, 3).reshape(NEXP * 128, 8 * DEXP)),
        "wd_r": f(np.asarray(inputs["w_down"][0]).reshape(NEXP, 4, 128, D).transpose(0, 2, 1, 3).reshape(NEXP * 128, 4 * D)),
    }
    j = np.arange(128)[:, None]; i = np.arange(128)[None, :]
    com["c_ident"] = f(np.eye(128)); com["c_causal"] = f(j <= i); com["c_ustrict"] = f(j < i)
    com["c_iota"] = f(np.arange(128)[:, None])
    import ml_dtypes
    bO = np.zeros((128, 12, 256), np.float64)
    c = np.arange(256)[None, :]
    for g in range(3):
        for h in range(4):
            sl = SLOPES[g * 4 + h] * DILS[g]
            dist = np.where(c < 128, c - j, c - 128 + 128 - j)
            valid = np.where(c < 128, (c - j) >= 0, j >= (c - 128))
            bO[:, g * 4 + h, :] = np.where(valid, -sl * dist, -30000.0)
    bf = lambda a: a.astype(np.float32).astype(ml_dtypes.bfloat16).astype(np.float64)
    bOh = bf(bO); bOl = bf(bO - bOh)
    com["c_bOh"] = f(bOh.reshape(128, -1)); com["c_bOl"] = f(bOl.reshape(128, -1))
    bHh1 = bOh[:, :, 128:256]; bHl1 = bOl[:, :, 128:256]
    bHh0 = np.full_like(bHh1, bf(np.array(-30000.0))); bHl0 = np.zeros_like(bHl1)
    maps = []
    for core in range(8):
        b, hf = core // 2, core % 2
        m = dict(com)
        m["xo"] = f(x[b, hf * NTOK:(hf + 1) * NTOK])
        m["xp"] = f(x[b, 0:NTOK]) if hf == 1 else np.zeros((NTOK, D), np.float32)
        m["c_bHh"] = f((bHh1 if hf == 1 else bHh0).reshape(128, -1)); m["c_bHl"] = f((bHl1 if hf == 1 else bHl0).reshape(128, -1))
        maps.append(m)
    return maps


_NC = {}


def kernel(**inputs):
    if "nc" not in _NC:
        _NC["nc"] = build()[0]
    maps = host_inputs(inputs)
    res = run_bass_kernel_spmd(_NC["nc"], maps, core_ids=list(range(8)))
    o = np.zeros((4, 8192, D), np.float32)
    for core in range(8):
        b, hf = core // 2, core % 2
        o[b, hf * NTOK:(hf + 1) * NTOK] = res.results[core]["out"]
    return o
```
